# Optimizing a Trainium2 kernel written in Bass

```python
import math
import jax, jax.numpy as jnp
from jax import lax
import numpy as np

D_MODEL = 1024
BATCH = 8
SEQ = 2048
DEPTH = 1

CHUNK = 64
Q_BLOCK = 128
N_MEM = 256
N_DIFF_HEADS = 4
DIFF_V_DIM = D_MODEL // 2 // N_DIFF_HEADS
DIFF_QK_DIM = DIFF_V_DIM // 2
N_FOX_HEADS = 4
FOX_HEAD_DIM = D_MODEL // 2 // N_FOX_HEADS
N_CROSS_HEADS = 4
CROSS_HEAD_DIM = D_MODEL // N_CROSS_HEADS
N_BUCKETS = 32
MAX_DISTANCE = 128
N_GROUPS = 4
EXPERTS_PER_GROUP = 4
N_EXPERTS = N_GROUPS * EXPERTS_PER_GROUP
TOP_K_IN_GROUP = 2
D_EXPERT = D_MODEL // 2
FORGET_BIAS_INIT = 2.0
EPS = 1e-6
NEG_INF = -1e30

DIFF_WIDTH = N_DIFF_HEADS * DIFF_V_DIM
FOX_WIDTH = N_FOX_HEADS * FOX_HEAD_DIM
MIX_WIDTH = DIFF_WIDTH + FOX_WIDTH
COL_SIZES = [
    N_DIFF_HEADS * 2 * DIFF_QK_DIM,
    N_DIFF_HEADS * 2 * DIFF_QK_DIM,
    DIFF_WIDTH,
    FOX_WIDTH,
    FOX_WIDTH,
    FOX_WIDTH,
    N_FOX_HEADS,
]
IN_COLS = sum(COL_SIZES)
SPLITS = [int(v) for v in np.cumsum(COL_SIZES)[:-1]]

kernel_name = "hybrid_diff_fox_memxattn_hmoe"


def rms_norm(x, g):
    xf = x.astype(jnp.float32)
    y = xf * lax.rsqrt(jnp.mean(xf * xf, axis=-1, keepdims=True) + EPS)
    return (y * g.astype(jnp.float32)).astype(x.dtype)


def t5_bucket(rel):
    nb = N_BUCKETS // 2
    max_exact = nb // 2
    ret = (rel > 0).astype(jnp.int32) * nb
    n = jnp.abs(rel)
    nf = jnp.maximum(n, 1).astype(jnp.float32)
    large = max_exact + (jnp.log(nf / max_exact) / math.log(MAX_DISTANCE / max_exact)
                         * (nb - max_exact)).astype(jnp.int32)
    large = jnp.minimum(large, nb - 1)
    return ret + jnp.where(n < max_exact, n, large)


def differential_attention(q, k, v, rel_bias, lam, lam_init, subln_g):
    s_len = q.shape[1]
    scale = DIFF_QK_DIM ** -0.5
    pos = jnp.arange(s_len, dtype=jnp.int32)
    chunk = pos // CHUNK
    outs = []
    for i in range(s_len // Q_BLOCK):
        q0, q1 = i * Q_BLOCK, (i + 1) * Q_BLOCK
        logits = jnp.einsum('bqhcd,bkhcd->bhcqk', q[:, q0:q1], k[:, :q1]).astype(jnp.float32) * scale
        rel = pos[None, :q1] - pos[q0:q1, None]
        bias = jnp.transpose(rel_bias[t5_bucket(rel)], (2, 0, 1)).astype(jnp.float32)
        mask = chunk[None, :q1] <= chunk[q0:q1, None]
        logits = jnp.where(mask, logits + bias[None, :, None], NEG_INF)
        probs = jax.nn.softmax(logits, axis=-1)
        attn = probs[:, :, 0] - lam * probs[:, :, 1]
        outs.append(jnp.einsum('bhqk,bkhe->bqhe', attn.astype(v.dtype), v[:, :q1]))
    o = jnp.concatenate(outs, axis=1)
    return rms_norm(o, subln_g) * (1.0 - lam_init)


def forgetting_attention(q, k, v, cum_logf, out_g):
    s_len = q.shape[1]
    scale = FOX_HEAD_DIM ** -0.5
    pos = jnp.arange(s_len, dtype=jnp.int32)
    outs = []
    for i in range(s_len // Q_BLOCK):
        q0, q1 = i * Q_BLOCK, (i + 1) * Q_BLOCK
        logits = jnp.einsum('bqhd,bkhd->bhqk', q[:, q0:q1], k[:, :q1]).astype(jnp.float32) * scale
        decay = cum_logf[:, :, q0:q1, None] - cum_logf[:, :, None, :q1]
        mask = pos[None, :q1] <= pos[q0:q1, None]
        logits = jnp.where(mask, logits + decay, NEG_INF)
        probs = jax.nn.softmax(logits, axis=-1)
        outs.append(jnp.einsum('bhqk,bkhd->bqhd', probs.astype(v.dtype), v[:, :q1]))
    o = jnp.concatenate(outs, axis=1)
    return rms_norm(o, out_g)


def memory_cross_attention(h, mem_n, w_cq, w_ckv, q_g, k_g, w_co):
    b, s_len, _ = h.shape
    q = (h @ w_cq).reshape(b, s_len, N_CROSS_HEADS, CROSS_HEAD_DIM)
    kv = (mem_n @ w_ckv).reshape(b, mem_n.shape[1], 2, N_CROSS_HEADS, CROSS_HEAD_DIM)
    q = rms_norm(q, q_g)
    k = rms_norm(kv[:, :, 0], k_g)
    v = kv[:, :, 1]
    logits = jnp.einsum('bshd,bmhd->bhsm', q, k).astype(jnp.float32) * CROSS_HEAD_DIM ** -0.5
    probs = jax.nn.softmax(logits, axis=-1)
    o = jnp.einsum('bhsm,bmhd->bshd', probs.astype(v.dtype), v)
    return o.reshape(b, s_len, D_MODEL) @ w_co


def hierarchical_moe(h, w_gr, b_gr, w_er, b_er, w_gate, w_up, w_down):
    b, s_len, d = h.shape
    t = h.reshape(-1, d)
    n_tok = t.shape[0]
    p_group = jax.nn.softmax((t @ w_gr + b_gr).astype(jnp.float32), axis=-1)
    g_idx = jnp.argmax(p_group, axis=-1)
    p_g = jnp.take_along_axis(p_group, g_idx[:, None], axis=-1)
    e_logits = (t @ w_er + b_er).astype(jnp.float32).reshape(n_tok, N_GROUPS, EXPERTS_PER_GROUP)
    sel = jnp.take_along_axis(e_logits, g_idx[:, None, None], axis=1)[:, 0]
    p_in = jax.nn.softmax(sel, axis=-1)
    top_v, top_i = lax.top_k(p_in, TOP_K_IN_GROUP)
    top_v = top_v / jnp.sum(top_v, axis=-1, keepdims=True)
    w = p_g * top_v
    eid = g_idx[:, None] * EXPERTS_PER_GROUP + top_i
    gates = jnp.sum(jax.nn.one_hot(eid, N_EXPERTS, dtype=jnp.float32) * w[..., None], axis=1)
    gates = gates.astype(t.dtype)
    y = jnp.zeros_like(t)
    for e in range(N_EXPERTS):
        a = jax.nn.silu(t @ w_gate[e]) * (t @ w_up[e])
        y = y + gates[:, e:e + 1] * (a @ w_down[e])
    return y.reshape(b, s_len, d)


def setup_inputs(seed: int = 0) -> dict:
    key = jax.random.key(seed)
    ks = iter(jax.random.split(key, 48))
    L, D = DEPTH, D_MODEL

    def nrm(shape, scale):
        return scale * jax.random.normal(next(ks), shape, jnp.float32)

    def gain(shape):
        return 1.0 + nrm(shape, 0.02)

    return {
        "x": nrm((BATCH, SEQ, D), 1.0),
        "mem": nrm((BATCH, N_MEM, D), 1.0),
        "rel_bias": nrm((N_BUCKETS, N_DIFF_HEADS), 0.5),
        "norm_mix_g": gain((L, D)),
        "w_in": nrm((L, D, IN_COLS), D ** -0.5),
        "b_forget": FORGET_BIAS_INIT + nrm((L, N_FOX_HEADS), 0.1),
        "diff_q_norm_g": gain((L, DIFF_QK_DIM)),
        "diff_k_norm_g": gain((L, DIFF_QK_DIM)),
        "diff_lambda_q1": nrm((L, DIFF_QK_DIM), 0.1),
        "diff_lambda_k1": nrm((L, DIFF_QK_DIM), 0.1),
        "diff_lambda_q2": nrm((L, DIFF_QK_DIM), 0.1),
        "diff_lambda_k2": nrm((L, DIFF_QK_DIM), 0.1),
        "diff_subln_g": gain((L, DIFF_V_DIM)),
        "fox_q_norm_g": gain((L, FOX_HEAD_DIM)),
        "fox_k_norm_g": gain((L, FOX_HEAD_DIM)),
        "fox_out_norm_g": gain((L, FOX_HEAD_DIM)),
        "w_out": nrm((L, MIX_WIDTH, D), MIX_WIDTH ** -0.5),
        "norm_cross_g": gain((L, D)),
        "norm_mem_g": gain((L, D)),
        "w_cq": nrm((L, D, D), D ** -0.5),
        "w_ckv": nrm((L, D, 2 * D), D ** -0.5),
        "cross_q_norm_g": gain((L, CROSS_HEAD_DIM)),
        "cross_k_norm_g": gain((L, CROSS_HEAD_DIM)),
        "w_co": nrm((L, D, D), D ** -0.5),
        "norm_ffn_g": gain((L, D)),
        "w_group_router": nrm((L, D, N_GROUPS), D ** -0.5),
        "b_group_router": nrm((L, N_GROUPS), 0.01),
        "w_expert_router": nrm((L, D, N_EXPERTS), D ** -0.5),
        "b_expert_router": nrm((L, N_EXPERTS), 0.01),
        "w_exp_gate": nrm((L, N_EXPERTS, D, D_EXPERT), D ** -0.5),
        "w_exp_up": nrm((L, N_EXPERTS, D, D_EXPERT), D ** -0.5),
        "w_exp_down": nrm((L, N_EXPERTS, D_EXPERT, D), D_EXPERT ** -0.5),
    }


def reference(x, mem, rel_bias, norm_mix_g, w_in, b_forget, diff_q_norm_g, diff_k_norm_g,
              diff_lambda_q1, diff_lambda_k1, diff_lambda_q2, diff_lambda_k2, diff_subln_g,
              fox_q_norm_g, fox_k_norm_g, fox_out_norm_g, w_out, norm_cross_g, norm_mem_g,
              w_cq, w_ckv, cross_q_norm_g, cross_k_norm_g, w_co, norm_ffn_g,
              w_group_router, b_group_router, w_expert_router, b_expert_router,
              w_exp_gate, w_exp_up, w_exp_down):
    b, s_len, _ = x.shape
    for l in range(DEPTH):
        h = rms_norm(x, norm_mix_g[l])
        proj = h @ w_in[l]
        dq, dk, dv, fq, fk, fv, f_logit = jnp.split(proj, SPLITS, axis=-1)
        dq = rms_norm(dq.reshape(b, s_len, N_DIFF_HEADS, 2, DIFF_QK_DIM), diff_q_norm_g[l])
        dk = rms_norm(dk.reshape(b, s_len, N_DIFF_HEADS, 2, DIFF_QK_DIM), diff_k_norm_g[l])
        dv = dv.reshape(b, s_len, N_DIFF_HEADS, DIFF_V_DIM)
        lam_init = 0.8 - 0.6 * math.exp(-0.3 * l)
        lam = (jnp.exp(jnp.sum(diff_lambda_q1[l].astype(jnp.float32) * diff_lambda_k1[l].astype(jnp.float32)))
               - jnp.exp(jnp.sum(diff_lambda_q2[l].astype(jnp.float32) * diff_lambda_k2[l].astype(jnp.float32)))
               + lam_init)
        o_diff = differential_attention(dq, dk, dv, rel_bias, lam, lam_init, diff_subln_g[l])

        fq = rms_norm(fq.reshape(b, s_len, N_FOX_HEADS, FOX_HEAD_DIM), fox_q_norm_g[l])
        fk = rms_norm(fk.reshape(b, s_len, N_FOX_HEADS, FOX_HEAD_DIM), fox_k_norm_g[l])
        fv = fv.reshape(b, s_len, N_FOX_HEADS, FOX_HEAD_DIM)
        log_f = jax.nn.log_sigmoid(f_logit.astype(jnp.float32) + b_forget[l].astype(jnp.float32))
        cum_logf = jnp.transpose(jnp.cumsum(log_f, axis=1), (0, 2, 1))
        o_fox = forgetting_attention(fq, fk, fv, cum_logf, fox_out_norm_g[l])

        mixed = jnp.concatenate([o_diff.reshape(b, s_len, DIFF_WIDTH),
                                 o_fox.reshape(b, s_len, FOX_WIDTH)], axis=-1)
        x = x + mixed @ w_out[l]

        x = x + memory_cross_attention(rms_norm(x, norm_cross_g[l]), rms_norm(mem, norm_mem_g[l]),
                                       w_cq[l], w_ckv[l], cross_q_norm_g[l], cross_k_norm_g[l], w_co[l])

        x = x + hierarchical_moe(rms_norm(x, norm_ffn_g[l]), w_group_router[l], b_group_router[l],
                                 w_expert_router[l], b_expert_router[l],
                                 w_exp_gate[l], w_exp_up[l], w_exp_down[l])
    return x
```

```python
import math
import numpy as np
import concourse.bass as bass
import concourse.mybir as mybir
from concourse.bass_utils import run_bass_kernel_spmd

F32 = mybir.dt.float32
BF16 = mybir.dt.bfloat16
AF = mybir.ActivationFunctionType
ALU = mybir.AluOpType
AX = mybir.AxisListType

S = 2048
D = 1024
NT = 16
NEG = -30000.0
EPS = 1e-6
ENGS = ["pe", "act", "dve", "pool", "sp"]


class Tracker:
    def __init__(self, nc, n_lanes=9, n_single=88):
        self.nc = nc
        self.engs = {"pe": nc.tensor, "act": nc.scalar, "dve": nc.vector,
                     "pool": nc.gpsimd, "sp": nc.sync}
        self.NL = n_lanes + n_single
        self.n_rr = n_lanes
        self.next_single = 4 + n_lanes
        self.eidx = {e: i for i, e in enumerate(ENGS)}
        self.sems = []
        self._ctx = []
        for i in range(4 + self.NL):
            cm = nc.semaphore(f"s{i}")
            self.sems.append(cm.__enter__())
            self._ctx.append(cm)
        self.NV = 4 + self.NL
        self.cnt = np.zeros(self.NV, dtype=np.int64)
        self.cur = {e: np.zeros(self.NV, dtype=np.int64) for e in ENGS}
        self.last_w = {}
        self.readers = {}
        self.lane_rr = 0
        self.n_waits = 0
        self.n_ops = 0

    def close(self):
        for cm in reversed(self._ctx):
            cm.__exit__(None, None, None)

    def _gather_deps(self, r, w):
        deps = []
        for k in r:
            lw = self.last_w.get(k)
            if lw is not None:
                deps.append(lw)
        for k in w:
            lw = self.last_w.get(k)
            if lw is not None:
                deps.append(lw)
            rd = self.readers.get(k)
            if rd:
                deps.extend(rd.values())
        return deps

    def _emit_waits(self, eng, deps, own_slot):
        cur = self.cur[eng]
        e = self.engs[eng]
        need = {}
        for (slot, c, vc) in deps:
            if slot == own_slot and slot < 4:
                if slot == 0:
                    continue
            if cur[slot] >= c:
                continue
            if need.get(slot, (0, None))[0] < c:
                need[slot] = (c, vc)
        items = sorted(need.items(), key=lambda kv: -kv[1][0])
        for slot, (c, vc) in items:
            if cur[slot] >= c:
                continue
            val = int(c) * (16 if slot >= 4 else 1)
            e.wait_ge(self.sems[slot], val)
            self.n_waits += 1
            np.maximum(cur, vc, out=cur)
            if cur[slot] < c:
                cur[slot] = c

    def _record(self, rec, r, w):
        for k in r:
            self.readers.setdefault(k, {})[rec[0]] = rec
        for k in w:
            self.last_w[k] = rec
            self.readers[k] = {}

    def op(self, eng, fn, r=(), w=()):
        slot = self.eidx[eng]
        deps = self._gather_deps(r, w)
        self._emit_waits(eng, deps, slot)
        inst = fn()
        self.cnt[slot] += 1
        c = int(self.cnt[slot])
        inst.then_inc(self.sems[slot], 1)
        vc = self.cur[eng].copy()
        vc[slot] = c
        rec = (slot, c, vc)
        self._record(rec, r, w)
        self.n_ops += 1
        return rec

    def dma(self, q, out, in_, r=(), w=(), **kw):
        if q == "pool":
            slot = self.next_single
            self.next_single += 1
            assert slot < self.NV, "out of single-use DMA semaphores"
        else:
            lane = self.lane_rr
            self.lane_rr = (self.lane_rr + 1) % self.n_rr
            slot = 4 + lane
        deps = self._gather_deps(r, w)
        prev = int(self.cnt[slot])
        if prev > 0:
            pvc = np.zeros(self.NV, dtype=np.int64)
            pvc[slot] = prev
            deps.append((slot, prev, pvc))
        self._emit_waits(q, deps, -1)
        inst = self.engs[q].dma_start(out=out, in_=in_, **kw)
        self.cnt[slot] += 1
        c = int(self.cnt[slot])
        inst.then_inc(self.sems[slot], 16)
        vc = self.cur[q].copy()
        vc[slot] = c
        rec = (slot, c, vc)
        self._record(rec, r, w)
        self.n_ops += 1
        return rec

    def barrier(self):
        full = self.cnt.copy()
        for eng in ENGS:
            cur = self.cur[eng]
            e = self.engs[eng]
            own = self.eidx[eng]
            for slot in range(self.NV):
                if slot == own and slot < 4:
                    continue
                if cur[slot] < full[slot]:
                    val = int(full[slot]) * (16 if slot >= 4 else 1)
                    e.wait_ge(self.sems[slot], val)
                    self.n_waits += 1
                    cur[slot] = full[slot]
        self.last_w.clear()
        self.readers.clear()

    def finish(self, eng="sp"):
        cur = self.cur[eng]
        e = self.engs[eng]
        for slot in range(self.NV):
            if cur[slot] < self.cnt[slot]:
                val = int(self.cnt[slot]) * (16 if slot >= 4 else 1)
                e.wait_ge(self.sems[slot], val)
                cur[slot] = self.cnt[slot]


def _t5_bucket_np(rel):
    nb = 16
    max_exact = 8
    ret = (rel > 0).astype(np.int32) * nb
    n = np.abs(rel)
    nf = np.maximum(n, 1).astype(np.float32)
    large = max_exact + (np.log(nf / np.float32(max_exact)) / np.float32(math.log(128 / max_exact))
                         * np.float32(nb - max_exact)).astype(np.int32)
    large = np.minimum(large, nb - 1)
    return ret + np.where(n < max_exact, n, large)


C_ID, C_BLK, C_ONE, C_TRI, C_SEL, C_MD, C_MF, C_G = 0, 128, 256, 384, 512, 640, 768, 896
C_W = 896 + 384


def _make_consts():
    c = np.zeros((128, C_W), dtype=np.float32)
    p = np.arange(128)[:, None]
    f = np.arange(128)[None, :]
    c[:, C_ID:C_ID + 128] = (p == f)
    c[:, C_BLK:C_BLK + 128] = ((p // 64) == (f // 64))
    c[:, C_ONE:C_ONE + 128] = 1.0
    c[:, C_TRI:C_TRI + 128] = (p <= f)
    c[:, C_SEL:C_SEL + 128] = (p == 127)
    c[:, C_MD:C_MD + 128] = np.where((p >= 64) & (f < 64), NEG, 0.0)
    c[:, C_MF:C_MF + 128] = np.where(p <= f, 0.0, NEG)
    u = np.arange(383)
    rel = u - 255
    bk = _t5_bucket_np(rel.astype(np.int32))
    g = np.zeros((32, 384), dtype=np.float32)
    g[bk, u] = 1.0
    c[0:32, C_G:C_G + 384] = g
    return c


def build_program():
    nc = bass.Bass("TRN2", target_bir_lowering=False)

    def din(name, shape):
        return nc.dram_tensor(name, list(shape), F32, kind="ExternalInput").ap()

    x_d = din("x", [S, D])
    mem_d = din("mem", [256, D])
    rel_bias_d = din("rel_bias", [32, 4])
    norm_mix_g_d = din("norm_mix_g", [D])
    w_in_d = din("w_in", [D, 3076])
    b_forget_d = din("b_forget", [4])
    dqg_d = din("diff_q_norm_g", [64])
    dkg_d = din("diff_k_norm_g", [64])
    lq1_d = din("diff_lambda_q1", [64])
    lk1_d = din("diff_lambda_k1", [64])
    lq2_d = din("diff_lambda_q2", [64])
    lk2_d = din("diff_lambda_k2", [64])
    dsub_d = din("diff_subln_g", [128])
    fqg_d = din("fox_q_norm_g", [128])
    fkg_d = din("fox_k_norm_g", [128])
    fog_d = din("fox_out_norm_g", [128])
    w_out_d = din("w_out", [D, D])
    norm_cross_g_d = din("norm_cross_g", [D])
    norm_mem_g_d = din("norm_mem_g", [D])
    w_cq_d = din("w_cq", [D, D])
    w_ckv_d = din("w_ckv", [D, 2 * D])
    cqg_d = din("cross_q_norm_g", [256])
    ckg_d = din("cross_k_norm_g", [256])
    w_co_d = din("w_co", [D, D])
    norm_ffn_g_d = din("norm_ffn_g", [D])
    w_gr_d = din("w_group_router", [D, 4])
    b_gr_d = din("b_group_router", [4])
    w_er_d = din("w_expert_router", [D, 16])
    b_er_d = din("b_expert_router", [16])
    w_eg_d = din("w_exp_gate", [16, D, 512])
    w_eu_d = din("w_exp_up", [16, D, 512])
    w_ed_d = din("w_exp_down", [16, 512, D])
    cst_d = din("cst", [128, C_W])
    out_d = nc.dram_tensor("out", [S, D], F32, kind="ExternalOutput").ap()

    T = Tracker(nc)
    V, A, PE = nc.vector, nc.scalar, nc.tensor
    LAM_INIT = 0.8 - 0.6 * math.exp(0.0)

    def wv(ap):
        return ap.rearrange("(c p) n -> p c n", p=128)

    from contextlib import ExitStack
    es = ExitStack()

    def sb(name, shape, dt=F32):
        return es.enter_context(nc.sbuf_tensor(name, list(shape), dt))

    cst = sb("cst_sb", [128, C_W])
    identb = sb("identb", [128, 128], BF16)
    blkb = sb("blkb", [128, 128], BF16)
    oneb = sb("oneb", [128, 128], BF16)
    gn = sb("gn", [128, 4, 8])
    pp = sb("pp", [128, 16])
    lams = sb("lams", [128, 8])
    b20 = sb("b20", [128, 20])
    sqb = [sb(f"sq{i}", [128, 512], BF16) for i in range(2)]
    rsb = [sb(f"rs{i}", [128, 512]) for i in range(2)]
    ET = [sb(f"ET{i}", [128, 512], BF16) for i in range(6)]
    qk = [sb(f"qk{i}", [128, 2, S], BF16) for i in range(2)]
    obn = sb("obn", [128, 4, 128], BF16)
    sm = sb("sm", [128, 32])
    wring = [sb(f"wr{i}", [128, 4096], BF16) for i in range(3)]
    rl = sb("rl", [128, 16, 20])
    gates = sb("gates", [128, 16, 16])
    rt = sb("rt", [128, 64])
    xn_bufs = [sb(f"xn{i}", [128, D], BF16) for i in range(2)]
    junk = sb("junk", [128, D], BF16)
    kTc = sb("kTc", [128, 4, 2, 256], BF16)
    vc = sb("vc", [128, 2, 4, 256], BF16)
    mixT = sb("mixT", [128, 8, S], BF16)
    hs = ExitStack()

    def sba(name, shape, dt=F32):
        return hs.enter_context(nc.sbuf_tensor(name, list(shape), dt))
    lamw = sba("lamw", [128, 4, 64])
    bfo = sba("bfo", [128, 4])
    rb = sba("rb", [32, 4])
    c15 = sba("c15", [128, 4])
    BT = sba("BT", [128, 4, 5, 128])
    fl = sba("fl", [128, 16, 4])
    cumL = sba("cumL", [128, 16, 4])
    carry = sba("carry", [128, 16, 4])
    cref = sba("cref", [128, 16, 4])
    fbt = sba("fbt", [128, 4, 16, 16])
    tmpn = [sba(f"tmpn{i}", [128, 512]) for i in range(2)]
    vb = [sba(f"vb{i}", [128, 16, 128], BF16) for i in range(2)]
    qz = [sba(f"qz{i}", [128, S], BF16) for i in range(2)]
    o1 = sba("o1", [128, 4, 128])
    ob = sba("ob", [128, 4, 128])
    rB = sba("rB", [128, 512])
    obB = sba("obB", [128, 4, 128])
    obnB = sba("obnB", [128, 4, 128], BF16)
    rB2 = [sba(f"rB2_{i}", [128, 512]) for i in range(2)]
    memT = sba("memT", [128, 8, 256], BF16)
    hT = sba("hT", [128, 8, S], BF16)
    xs = [sba(f"xs{i}", [128, D], F32) for i in range(2)]

    ps = [es.enter_context(nc.psum_tensor(f"ps{i}", [128, 512], F32)) for i in range(8)]
    psT = ps[7][:].bitcast(BF16)
    gb_cnt = [0]

    gb_list = [0, 1, 2, 6, 7]

    def next_gbank():
        b = gb_list[gb_cnt[0] % len(gb_list)]
        gb_cnt[0] += 1
        return b

    def bk(i):
        return ("bank", i)

    T.dma("sp", cst[:], cst_d[:, :], w=["cst"])
    T.op("dve", lambda: V.tensor_copy(out=identb[:], in_=cst[:, C_ID:C_ID + 128]), r=["cst"], w=["identb"])
    T.op("dve", lambda: V.tensor_copy(out=blkb[:], in_=cst[:, C_BLK:C_BLK + 128]), r=["cst"], w=["blkb"])
    T.op("dve", lambda: V.tensor_copy(out=oneb[:], in_=cst[:, C_ONE:C_ONE + 128]), r=["cst"], w=["oneb"])
    bt_chunks = []
    for t in range(2):
        for f0 in range(0, 128, 16):
            def mmb(t=t, f0=f0):
                if t == 1 and f0 >= 96:
                    for h in range(4):
                        T.op("dve", lambda h=h: V.tensor_copy(out=BT[:, h, 1, f0:f0 + 16], in_=c15[:, h:h + 1].broadcast_to([128, 16])),
                             r=["c15"], w=[("BT", h)])
                    return
                gbk = next_gbank()

                def mm():
                    for f in range(f0, f0 + 16):
                        u0 = 255 - f - 128 * t
                        i = PE.matmul(ps[gbk][:, 4 * (f - f0):4 * (f - f0) + 4], lhsT=cst[0:32, C_G + u0:C_G + u0 + 128],
                                      rhs=rb[:, :], start=True, stop=True)
                    return i
                T.op("pe", mm, r=["cst", "rb"], w=[bk(gbk)])
                for h in range(4):
                    src = ps[gbk][:, 0:64].rearrange("p (f h) -> p f h", h=4)[:, :, h]
                    T.op("dve", lambda h=h, src=src: V.tensor_copy(out=BT[:, h, t, f0:f0 + 16], in_=src), w=[bk(gbk), ("BT", h)])
            bt_chunks.append(mmb)

    def bt_finish():
        for h in range(4):
            T.op("dve", lambda h=h: V.tensor_tensor(out=BT[:, h, 0, :], in0=BT[:, h, 0, :], in1=cst[:, C_MD:C_MD + 128], op=ALU.add),
                 r=["cst"], w=[("BT", h)])
            T.op("dve", lambda h=h: V.tensor_copy(out=BT[:, h, 2:5, :].rearrange("p t q -> p (t q)"),
                                                  in_=c15[:, h:h + 1].broadcast_to([128, 384])),
                 r=["c15"], w=[("BT", h)])

    def rstd_small(src_ss, n, scale, keyr, dst, keyw):
        T.op("act", lambda: A.activation(out=dst, in_=src_ss, func=AF.Ln, scale=scale, bias=EPS), r=[keyr], w=[keyw])
        T.op("act", lambda: A.activation(out=dst, in_=dst, func=AF.Exp, scale=-0.5), r=[keyw], w=[keyw])

    nrm_cnt = [0]

    gB = qk[1][:, 0, :].bitcast(F32)
    GBK = [("qk", 1, 0, tg) for tg in range(4)]

    def norm_stage1(src_ap, src_keys, n):
        b = n % 2
        sc = sm[:, 16 + 2 * b:16 + 2 * b + 1]
        sr = sm[:, 17 + 2 * b:17 + 2 * b + 1]
        T.op("act", lambda: A.activation(out=junk[:], in_=src_ap, func=AF.Square, accum_out=sc), r=src_keys, w=["junk", ("nss", b)])
        rstd_small(sc, 1, 1.0 / D, ("nss", b), sr, ("nrs", b))
        T.op("dve", lambda: V.scalar_tensor_tensor(out=xn_bufs[b][:], in0=src_ap, scalar=sr, in1=gB, op0=ALU.mult, op1=ALU.mult),
             r=list(src_keys) + [("nrs", b)] + GBK, w=[("xn", b)])

    def norm_stage2(n, dstT, dkey, t):
        b = n % 2
        nb_ = 6 + b
        pT = ps[nb_][:].bitcast(BF16)

        def tr():
            for c in range(8):
                i = PE.transpose(out=pT[:, c * 128:(c + 1) * 128], in_=xn_bufs[b][:, c * 128:(c + 1) * 128], identity=identb[:])
            return i
        T.op("pe", tr, r=[("xn", b), "identb"], w=[bk(nb_)])
        T.op("dve", lambda: V.tensor_copy(out=dstT[:, :, t * 128:(t + 1) * 128], in_=pT.rearrange("p (c n) -> p c n", n=128)),
             w=[bk(nb_)] + [(dkey, t // 4, c) for c in range(8)])

    def norm_all(items, g_d, dstT, dkey):
        T.dma("sp", gB, g_d.partition_broadcast(128), w=GBK)
        prev = None
        for (t, src_ap, src_keys, pre) in items:
            if pre is not None:
                pre()
            n = nrm_cnt[0]
            nrm_cnt[0] += 1
            norm_stage1(src_ap, src_keys, n)
            if prev is not None:
                norm_stage2(prev[0], dstT, dkey, prev[1])
            prev = (n, t)
        norm_stage2(prev[0], dstT, dkey, prev[1])

    qkn_cnt = [0]

    def emit_qknorm(banks, dk, gcols, dsts, rkeys, wkeys, blk_ap, n=512, ssbank=None, defer=None, split=None):
        i0 = qkn_cnt[0]
        qkn_cnt[0] += 1
        sqs = []
        for bi, bnk in enumerate(banks):
            sq = sqb[i0 % 2] if len(banks) == 1 else sqb[bi]
            sqs.append(sq)
            T.op("act", lambda bnk=bnk, sq=sq: A.activation(out=sq[:, 0:n], in_=ps[bnk][:, 0:n], func=AF.Square),
                 w=[bk(bnk), ("sq", id(sq))])

        def part_b():
            _qknorm_b(banks, dk, gcols, dsts, rkeys, wkeys, blk_ap, n, (next_gbank() if ssbank is None else ssbank), sqs, i0, split)
        if defer is not None:
            defer.append(part_b)
        else:
            part_b()

    def _qknorm_b(banks, dk, gcols, dsts, rkeys, wkeys, blk_ap, n, ssbank, sqs, i0, split):
        def mss():
            for bi, sq in enumerate(sqs):
                i = PE.matmul(ps[ssbank][:, 0:n], lhsT=blk_ap, rhs=sq[:, 0:n], start=(bi == 0), stop=(bi == len(sqs) - 1))
            return i
        T.op("pe", mss, r=[("sq", id(sq)) for sq in sqs] + ["blkb", "oneb"], w=[bk(ssbank)])
        rs = rsb[i0 % 2]
        T.op("act", lambda: A.activation(out=rs[:, 0:n], in_=ps[ssbank][:, 0:n], func=AF.Ln, scale=1.0 / dk, bias=EPS),
             w=[bk(ssbank), ("rs", i0 % 2)])
        T.op("act", lambda: A.activation(out=rs[:, 0:n], in_=rs[:, 0:n], func=AF.Exp, scale=-0.5), w=[("rs", i0 % 2)])
        if split is not None:
            bnk = banks[0]
            for (lo, hi, dst, wk) in split:
                T.op("dve", lambda lo=lo, hi=hi, dst=dst: V.scalar_tensor_tensor(out=dst, in0=ps[bnk][lo:hi, 0:n], scalar=gcols[0][lo:hi],
                                                                                  in1=rs[lo:hi, 0:n], op0=ALU.mult, op1=ALU.mult),
                     r=[("rs", i0 % 2)] + list(rkeys), w=[bk(bnk)] + list(wk))
            return
        for bi, bnk in enumerate(banks):
            T.op("dve", lambda bi=bi, bnk=bnk: V.scalar_tensor_tensor(out=dsts[bi], in0=ps[bnk][:, 0:n], scalar=gcols[bi],
                                                                       in1=rs[:, 0:n], op0=ALU.mult, op1=ALU.mult),
                 r=[("rs", i0 % 2)] + list(rkeys), w=[bk(bnk)] + list(wkeys[bi]))

    def pre_a(t):
        def f():
            T.dma("sp", xs[t % 2][:], x_d[t * 128:(t + 1) * 128, :], w=[("xs", t % 2)])
        return f
    norm_all([(t, xs[t % 2][:], [("xs", t % 2)], pre_a(t)) for t in range(NT)], norm_mix_g_d, hT, "hT")

    wq_early = wring[0][:, 0:3072].rearrange("p (c n) -> p c n", n=384)
    for i_, c0_ in enumerate((1536, 2048, 2560)):
        T.dma("pool", wq_early[:, :, i_ * 128:(i_ + 1) * 128], wv(w_in_d)[:, :, c0_:c0_ + 128], w=[("wr", 0)])
    def col(ap):
        return ap.rearrange("(p o) -> p o", o=1)
    T.dma("sp", pp[0:64, 0:1], col(dqg_d), w=["pp0"], allow_slow_non_contiguous=True)
    T.dma("sp", pp[64:128, 0:1], col(dqg_d), w=["pp0"], allow_slow_non_contiguous=True)
    T.dma("sp", pp[0:64, 1:2], col(dkg_d), w=["pp1"], allow_slow_non_contiguous=True)
    T.dma("sp", pp[64:128, 1:2], col(dkg_d), w=["pp1"], allow_slow_non_contiguous=True)
    T.dma("sp", pp[:, 2:3], col(fqg_d), w=["pp2"], allow_slow_non_contiguous=True)
    T.dma("sp", pp[:, 3:4], col(fkg_d), w=["pp3"], allow_slow_non_contiguous=True)
    T.dma("sp", pp[:, 4:5], col(dsub_d), w=["pp4"], allow_slow_non_contiguous=True)
    T.dma("sp", pp[:, 5:6], col(fog_d), w=["pp5"], allow_slow_non_contiguous=True)
    T.dma("sp", pp[:, 6:8], cqg_d.rearrange("(c p) -> p c", p=128), w=["pp6"], allow_slow_non_contiguous=True)
    T.dma("sp", pp[:, 8:10], ckg_d.rearrange("(c p) -> p c", p=128), w=["pp8"], allow_slow_non_contiguous=True)
    T.op("dve", lambda: V.tensor_scalar(out=pp[:, 0:1], in0=pp[:, 0:1], scalar1=64 ** -0.5, scalar2=None, op0=ALU.mult), r=["pp0"], w=["pp0"])
    T.op("dve", lambda: V.tensor_scalar(out=pp[:, 2:3], in0=pp[:, 2:3], scalar1=128 ** -0.5, scalar2=None, op0=ALU.mult), r=["pp2"], w=["pp2"])
    T.op("dve", lambda: V.tensor_scalar(out=pp[:, 4:5], in0=pp[:, 4:5], scalar1=1.0 - LAM_INIT, scalar2=None, op0=ALU.mult), r=["pp4"], w=["pp4"])
    T.op("dve", lambda: V.tensor_scalar(out=pp[:, 6:8], in0=pp[:, 6:8], scalar1=256 ** -0.5, scalar2=None, op0=ALU.mult), r=["pp6"], w=["pp6"])
    for i, l_d in enumerate([lq1_d, lk1_d, lq2_d, lk2_d]):
        T.dma("sp", lamw[:, i, :], l_d.partition_broadcast(128), w=[("lamw", i)])
    T.op("dve", lambda: V.tensor_tensor(out=lamw[:, 0, :], in0=lamw[:, 0, :], in1=lamw[:, 1, :], op=ALU.mult), r=[("lamw", 1)], w=[("lamw", 0)])
    T.op("dve", lambda: V.tensor_tensor(out=lamw[:, 2, :], in0=lamw[:, 2, :], in1=lamw[:, 3, :], op=ALU.mult), r=[("lamw", 3)], w=[("lamw", 2)])
    T.op("dve", lambda: V.reduce_sum(out=lams[:, 0:1], in_=lamw[:, 0, :], axis=AX.X), r=[("lamw", 0)], w=["lams0"])
    T.op("dve", lambda: V.reduce_sum(out=lams[:, 1:2], in_=lamw[:, 2, :], axis=AX.X), r=[("lamw", 2)], w=["lams1"])
    T.op("act", lambda: A.activation(out=lams[:, 2:4], in_=lams[:, 0:2], func=AF.Exp), r=["lams0", "lams1"], w=["lams2"])
    T.op("dve", lambda: V.tensor_tensor(out=lams[:, 4:5], in0=lams[:, 2:3], in1=lams[:, 3:4], op=ALU.subtract), r=["lams2"], w=["lams4"])
    T.op("dve", lambda: V.tensor_scalar(out=lams[:, 5:6], in0=lams[:, 4:5], scalar1=-1.0, scalar2=-LAM_INIT, op0=ALU.mult, op1=ALU.add), r=["lams4"], w=["neglam"])
    T.dma("sp", bfo[:], b_forget_d.partition_broadcast(128), w=["bfo"])
    T.dma("sp", b20[:, 0:4], b_gr_d.partition_broadcast(128), w=["b20a"])
    T.dma("sp", b20[:, 4:20], b_er_d.partition_broadcast(128), w=["b20b"])
    T.dma("sp", rb[:], rel_bias_d[:, :], w=["rb"])
    T.dma("sp", c15[:], rel_bias_d[15:16, :].partition_broadcast(128).rearrange("p o h -> p (o h)"), w=["c15"])


    def hT_keys(tg):
        return [("hT", tg, c) for c in range(8)]

    wf = wring[2]
    T.dma("pool", wf[:, 0:32].rearrange("p (c n) -> p c n", n=4), wv(w_in_d)[:, :, 3072:3076], w=[("wr", 2)],
          allow_slow_non_contiguous=True)

    def mmf():
        for t in range(NT):
            for c in range(8):
                i = PE.matmul(ps[5][:, 4 * t:4 * t + 4], lhsT=hT[:, c, t * 128:(t + 1) * 128], rhs=wf[:, 4 * c:4 * c + 4],
                              start=(c == 0), stop=(c == 7))
        return i
    T.op("pe", mmf, r=[("wr", 2)] + [k for tg in range(4) for k in hT_keys(tg)], w=[bk(5)])
    for h in range(4):
        src = ps[5][:, 0:64].rearrange("p (t h) -> p t h", h=4)[:, :, h]
        T.op("dve", lambda h=h, src=src: V.tensor_scalar(out=fl[:, :, h], in0=src, scalar1=bfo[:, h:h + 1], scalar2=None, op0=ALU.add),
             r=["bfo"], w=[bk(5), "fl"])
    flf = fl[:].rearrange("p t h -> p (t h)")
    T.op("act", lambda: A.activation(out=flf, in_=flf, func=AF.Exp, scale=-1.0), w=["fl"])
    T.op("act", lambda: A.activation(out=flf, in_=flf, func=AF.Ln, bias=1.0), w=["fl"])
    T.op("pe", lambda: PE.matmul(ps[5][:, 0:64], lhsT=cst[:, C_TRI:C_TRI + 128], rhs=flf, start=True, stop=True),
         r=["fl", "cst"], w=[bk(5)])
    T.op("pe", lambda: PE.matmul(ps[6][:, 0:64], lhsT=cst[:, C_ONE:C_ONE + 128], rhs=flf, start=True, stop=True),
         r=["fl", "cst"], w=[bk(6)])
    T.op("dve", lambda: V.memset(carry[:, 0, :], 0.0), w=["carry"])
    totv = ps[6][:, 0:64].rearrange("p (t h) -> p t h", h=4)
    for j in range(1, NT):
        T.op("dve", lambda j=j: V.tensor_tensor(out=carry[:, j, :], in0=carry[:, j - 1, :], in1=totv[:, j - 1, :], op=ALU.add),
             w=["carry", bk(6)])
    T.op("dve", lambda: V.tensor_tensor(out=cumL[:].rearrange("p t h -> p (t h)"), in0=ps[5][:, 0:64],
                                        in1=carry[:].rearrange("p t h -> p (t h)"), op=ALU.add), r=["carry"], w=[bk(5), "cumL"])
    T.op("pe", lambda: PE.matmul(ps[5][:, 0:64], lhsT=cst[:, C_SEL:C_SEL + 128], rhs=cumL[:].rearrange("p t h -> p (t h)"),
                                 start=True, stop=True), r=["cumL", "cst"], w=[bk(5)])
    T.op("dve", lambda: V.tensor_scalar(out=cref[:].rearrange("p t h -> p (t h)"), in0=ps[5][:, 0:64], scalar1=-1.0, scalar2=None,
                                        op0=ALU.mult), w=[bk(5), "cref"])
    for h in range(4):
        T.op("dve", lambda h=h: V.tensor_tensor(out=fbt[:, h, :, :], in0=cumL[:, :, h].unsqueeze(2).broadcast_to([128, NT, NT]),
                                                in1=cref[:, :, h].unsqueeze(1).broadcast_to([128, NT, NT]), op=ALU.add),
             r=["cref", "cumL"], w=[("fbt", h)])

    def pre_m(mt):
        return lambda: T.dma("sp", xs[mt][:], mem_d[mt * 128:(mt + 1) * 128, :], w=[("xs", mt)])
    norm_all([(mt, xs[mt][:], [("xs", mt)], pre_m(mt)) for mt in range(2)], norm_mem_g_d, memT, "memT")
    memkeys = [("memT", 0, c) for c in range(8)]
    def mem_piece(piece):
        T.dma("pool", wring[2][:].rearrange("p (c n) -> p c n", n=512), wv(w_ckv_d)[:, :, piece * 512:(piece + 1) * 512], w=[("wr", 2)])
        wsl = wring[2][:].rearrange("p (c n) -> p c n", n=512)
        if piece < 2:
            for hh in range(2):
                h = piece * 2 + hh

                def mmk(hh=hh):
                    for b in range(2):
                        for c in range(8):
                            i = PE.matmul(ps[b][:, 0:256], lhsT=wsl[:, c, hh * 256 + b * 128:hh * 256 + (b + 1) * 128], rhs=memT[:, c, :],
                                          start=(c == 0), stop=(c == 7))
                    return i
                T.op("pe", mmk, r=[("wr", 2)] + memkeys, w=[bk(0), bk(1)])
                emit_qknorm([0, 1], 256, [pp[:, 8:9], pp[:, 9:10]], [kTc[:, h, 0, :], kTc[:, h, 1, :]], ["pp8"],
                            [[("kTc", h)], [("kTc", h)]], oneb[:], n=256, ssbank=2)
        else:
            half = piece - 2
            for mt in range(2):
                def mmv(mt=mt):
                    for c in range(8):
                        i = PE.matmul(ps[mt][:], lhsT=memT[:, c, mt * 128:(mt + 1) * 128], rhs=wsl[:, c, :], start=(c == 0), stop=(c == 7))
                    return i
                T.op("pe", mmv, r=[("wr", 2)] + memkeys, w=[bk(mt)])
                T.op("dve", lambda mt=mt, half=half: V.tensor_copy(out=vc[:, mt, 2 * half:2 * half + 2, :],
                                                                  in_=ps[mt][:].rearrange("p (a b) -> p a b", b=256)),
                     w=[bk(mt), ("vc", mt, half)])
    mem_chunks = [(lambda p=p: mem_piece(p)) for p in range(4)]

    for i in range(2):
        T.op("pool", lambda i=i: nc.gpsimd.memset(qk[i][64:128, 0, :], 0.0), w=[("qk", i, 0, tg) for tg in range(4)])
        T.op("pool", lambda i=i: nc.gpsimd.memset(qz[i][0:64, :], 0.0), w=[("qz", i, tg) for tg in range(4)])


    def head_cols(H):
        if H < 4:
            return (H * 128, 512 + H * 128, 1024 + H * 128)
        h = H - 4
        return (1536 + h * 128, 2048 + h * 128, 2560 + h * 128)

    def proj_chunks(H, pbanks=(5,)):
        slot = H % 2
        wsl = wring[slot]
        cq, ck, cv = head_cols(H)
        wq = wsl[:, 0:3072].rearrange("p (c n) -> p c n", n=384)
        chunks = []

        isdiff = H < 4

        def load():
            for i, c0 in enumerate((cq, ck, cv)):
                T.dma("pool", wq[:, :, i * 128:(i + 1) * 128], wv(w_in_d)[:, :, c0:c0 + 128], w=[("wr", slot)])
            if isdiff:
                T.op("pool", lambda: nc.gpsimd.memset(qk[slot][64:128, 0, :], 0.0), w=[("qk", slot, 0, tg) for tg in range(4)])
        chunks.append(load)
        dk = 64 if isdiff else 128
        blk_ap = blkb[:] if isdiff else oneb[:]
        for which in range(2):
            gcol = pp[:, (0 if isdiff else 2) + which:(0 if isdiff else 2) + which + 1]
            gkey = f"pp{(0 if isdiff else 2) + which}"
            for tg in range(4):
                pb = pbanks[(which * 4 + tg) % len(pbanks)]

                def ch(which=which, tg=tg, gcol=gcol, gkey=gkey, holder=None, pb=pb):
                    def mm():
                        for c in range(8):
                            i = PE.matmul(ps[pb][:], lhsT=wq[:, c, which * 128:(which + 1) * 128], rhs=hT[:, c, tg * 512:(tg + 1) * 512],
                                          start=(c == 0), stop=(c == 7))
                        return i
                    T.op("pe", mm, r=[("wr", slot)] + hT_keys(tg), w=[bk(pb)])
                    cs_ = slice(tg * 512, (tg + 1) * 512)
                    sp = None
                    if isdiff and which == 0:
                        sp = [(0, 64, qk[slot][0:64, 0, cs_], [("qk", slot, 0, tg)]),
                              (64, 128, qz[slot][64:128, cs_], [("qz", slot, tg)])]
                    emit_qknorm([pb], dk, [gcol], [qk[slot][:, which, cs_]], [gkey],
                                [[("qk", slot, which, tg)]], blk_ap, defer=holder, split=sp)
                holder = []
                ch.__defaults__ = ch.__defaults__
                chunks.append((lambda ch=ch, holder=holder: ch(holder=holder)))
                chunks.append((lambda holder=holder: holder.pop(0)()))
        for tg in range(4):
            def chv(tg=tg):
                def mm():
                    for tt in range(4):
                        t = tg * 4 + tt
                        for c in range(8):
                            i = PE.matmul(ps[5][:, tt * 128:(tt + 1) * 128], lhsT=hT[:, c, t * 128:(t + 1) * 128],
                                          rhs=wq[:, c, 256:384], start=(c == 0), stop=(c == 7))
                    return i
                T.op("pe", mm, r=[("wr", slot)] + hT_keys(tg), w=[bk(5)])
                T.op("dve", lambda: V.tensor_copy(out=vb[slot][:, tg * 4:(tg + 1) * 4, :],
                                                  in_=ps[5][:].rearrange("p (a b) -> p a b", b=128)),
                     w=[bk(5), ("vb", slot, tg)])
            chunks.append(chv)
        return chunks

    st_cnt = [0]
    fin_cnt = [0]

    def attention(H, pending, dstT):
        slot = H % 2
        isdiff = H < 4
        h = H if isdiff else H - 4
        qT = qk[slot][:, 0, :]
        kT = qk[slot][:, 1, :]
        vv = vb[slot]
        comps = [0, 1] if isdiff else [0]
        steps = [(I, c, j) for I in range(4) for c in comps for j in range(4 * I + 4)]

        def acc_ap(il):
            return ps[3 + il // 2][:, (il % 2) * 256:(il % 2) * 256 + 129]

        def emit_S(n):
            I, c, j = steps[n]
            i0 = max(4 * I, j)
            ncol = (4 * I + 4 - i0) * 128
            q0 = i0 * 128
            sbank = next_gbank()
            etb = st_cnt[0] % 6
            st_cnt[0] += 1
            et = ET[etb]
            if isdiff and c == 1:
                rk = [("qz", slot, I), ("qk", slot, 1, j // 4)]
                qsrc = qz[slot]
            else:
                rk = [("qk", slot, 0, I), ("qk", slot, 1, j // 4)]
                qsrc = qT
            T.op("pe", lambda: PE.matmul(ps[sbank][:, 0:ncol], lhsT=kT[:, j * 128:(j + 1) * 128], rhs=qsrc[:, q0:q0 + ncol],
                                         start=True, stop=True), r=rk, w=[bk(sbank)])
            nb = ncol // 128
            late = []
            if isdiff:
                t_lo = i0 - j
                n_near = 0 if t_lo >= 2 else min(nb, 2 - t_lo)
                if n_near > 0:
                    tb = tmpn[n % 2]
                    tkey = ("tmpn", n % 2)
                    bsrc = BT[:, h, t_lo:t_lo + nb, :].rearrange("p t q -> p (t q)")
                    T.op("dve", lambda: V.tensor_tensor(out=tb[:, 0:ncol], in0=ps[sbank][:, 0:ncol], in1=bsrc, op=ALU.add),
                         r=[("BT", h)], w=[bk(sbank), tkey])
                    late.append((lambda: A.activation(out=et[:, 0:ncol], in_=tb[:, 0:ncol], func=AF.Exp), [tkey]))
                else:
                    T.op("act", lambda: A.activation(out=et[:, 0:ncol], in_=ps[sbank][:, 0:ncol], func=AF.Exp, bias=c15[:, h:h + 1]),
                         r=["c15"], w=[bk(sbank), ("ET", etb)])
            else:
                runs = []
                for bi in range(nb):
                    i = i0 + bi
                    pr = i // 2
                    if i == j:
                        cs = slice(bi * 128, (bi + 1) * 128)
                        tb = tmpn[n % 2]
                        tkey = ("tmpn", n % 2)
                        T.op("dve", lambda cs=cs, tb=tb: V.tensor_tensor(out=tb[:, 0:128], in0=ps[sbank][:, cs], in1=cst[:, C_MF:C_MF + 128], op=ALU.add),
                             r=["cst"], w=[bk(sbank), tkey])
                        late.append((lambda cs=cs, tb=tb, pr=pr: A.activation(out=et[:, cs], in_=tb[:, 0:128], func=AF.Exp, bias=fbt[:, h, j, 2 * pr:2 * pr + 1]),
                                     [tkey, ("fbt", h)]))
                    elif runs and runs[-1][2] == pr and runs[-1][1] == bi * 128:
                        runs[-1] = (runs[-1][0], (bi + 1) * 128, pr)
                    else:
                        runs.append((bi * 128, (bi + 1) * 128, pr))
                for (lo_, hi_, pr) in runs:
                    T.op("act", lambda lo_=lo_, hi_=hi_, pr=pr: A.activation(out=et[:, lo_:hi_], in_=ps[sbank][:, lo_:hi_], func=AF.Exp,
                                                                            bias=fbt[:, h, j, 2 * pr:2 * pr + 1]),
                         r=[("fbt", h)], w=[bk(sbank), ("ET", etb)])
            for fn, rk_ in late:
                T.op("act", fn, r=rk_, w=[("ET", etb)])
            return (etb, i0, nb)

        def accb(I, c):
            return (3, 4)

        def emit_AV(n, info):
            I, c, j = steps[n]
            etb, i0, nb = info
            et = ET[etb]
            ncol = nb * 128
            c0 = (i0 - 4 * I) * 128
            first = (j == 0)
            last = (j == 4 * I + 3)

            bA, bB = accb(I, c)

            def mm():
                PE.matmul(ps[bA][:, c0:c0 + ncol], lhsT=vv[:, j, :], rhs=et[:, 0:ncol], start=first, stop=last)
                return PE.matmul(ps[bB][:, c0:c0 + ncol], lhsT=oneb[:], rhs=et[:, 0:ncol], start=first, stop=last)
            T.op("pe", mm, r=[("ET", etb), ("vb", slot, j // 4), "oneb"], w=[bk(bA), bk(bB)])
            if last:
                finalize(I, c)

        o1f = o1[:].rearrange("p a b -> p (a b)")
        obfs = [ob[:].rearrange("p a b -> p (a b)"), obB[:].rearrange("p a b -> p (a b)")]
        obnfs = [obn[:].rearrange("p a b -> p (a b)"), obnB[:].rearrange("p a b -> p (a b)")]

        def finalize(I, c):
            bA, bB = accb(I, c)
            T.op("act", lambda: A.activation(out=rB[:], in_=ps[bB][:], func=AF.Ln), w=[bk(bB), "rB"])
            T.op("act", lambda: A.activation(out=rB[:], in_=rB[:], func=AF.Exp, scale=-1.0), w=["rB"])
            if isdiff and c == 0:
                T.op("dve", lambda: V.tensor_tensor(out=o1f, in0=ps[bA][:], in1=rB[:], op=ALU.mult), r=["rB"], w=[bk(bA), "o1"])
                return
            par = fin_cnt[0] % 2
            fin_cnt[0] += 1
            obf, obnf, rr = obfs[par], obnfs[par], rB2[par]
            T.op("dve", lambda: V.tensor_tensor(out=obf, in0=ps[bA][:], in1=rB[:], op=ALU.mult), r=["rB"], w=[bk(bA), ("ob", par)])
            if isdiff:
                T.op("dve", lambda: V.scalar_tensor_tensor(out=obf, in0=obf, scalar=lams[:, 5:6], in1=o1f, op0=ALU.mult, op1=ALU.add),
                     r=["neglam", "o1"], w=[("ob", par)])
            T.op("dve", lambda: V.tensor_tensor(out=obnf, in0=obf, in1=obf, op=ALU.mult), r=[("ob", par)], w=[("obn", par)])

            def part2():
                sbk = next_gbank()
                T.op("pe", lambda: PE.matmul(ps[sbk][:], lhsT=oneb[:], rhs=obnf, start=True, stop=True), r=[("obn", par), "oneb"], w=[bk(sbk)])
                T.op("act", lambda: A.activation(out=rr[:], in_=ps[sbk][:], func=AF.Ln, scale=1.0 / 128, bias=EPS), w=[bk(sbk), ("rB2", par)])
                T.op("act", lambda: A.activation(out=rr[:], in_=rr[:], func=AF.Exp, scale=-0.5), w=[("rB2", par)])
                gcol = pp[:, 4:5] if isdiff else pp[:, 5:6]
                T.op("dve", lambda: V.scalar_tensor_tensor(out=dstT[:, H, I * 512:(I + 1) * 512], in0=obf, scalar=gcol, in1=rr[:],
                                                           op0=ALU.mult, op1=ALU.mult),
                     r=[("ob", par), ("rB2", par), "pp4", "pp5"], w=[("mixT", H, I)])
            deferred.append([6, part2])

        return len(steps), emit_S, emit_AV

    deferred = []

    order = [4, 5, 6, 7, 0, 1, 2, 3]
    pc0 = proj_chunks(order[0], pbanks=(5, 3, 4))
    qa = [pc0[1 + 2 * k] for k in range(8)]
    qb = [pc0[2 + 2 * k] for k in range(8)]
    qa[0]()
    for k in range(1, 8):
        qa[k]()
        qb[k - 1]()
    qb[7]()
    for ch in pc0[17:]:
        ch()
    heads = {}
    gsteps = []
    pend_of = {}
    for oi, H in enumerate(order):
        nst_h, eS, eAV = attention(H, None, mixT)
        heads[H] = (eS, eAV)
        gsteps += [(H, n) for n in range(nst_h)]
        pend = proj_chunks(order[oi + 1]) if oi < 7 else []
        if oi < 4:
            if oi < 2:
                extra = bt_chunks[8 * oi:8 * oi + 8] + ([bt_finish] if oi == 1 else [])
            else:
                extra = mem_chunks[2 * (oi - 2):2 * (oi - 2) + 2]
            merged = [pend.pop(0)]
            while pend or extra:
                if pend:
                    merged.append(pend.pop(0))
                if pend:
                    merged.append(pend.pop(0))
                if extra:
                    merged.append(extra.pop(0))
            pend = merged
        pend_of[H] = (pend, max(1, nst_h // (len(pend) + 1)) if pend else nst_h)
    infos = {}
    DEPTH = 3
    curH = None
    for g in range(len(gsteps) + DEPTH):
        if g < len(gsteps):
            H, n = gsteps[g]
            if H != curH:
                if curH is not None:
                    while pend_of[curH][0]:
                        pend_of[curH][0].pop(0)()
                curH = H
            infos[g] = heads[H][0](n)
        if g >= DEPTH:
            H2, n2 = gsteps[g - DEPTH]
            heads[H2][1](n2, infos.pop(g - DEPTH))
        for d in list(deferred):
            d[0] -= 1
            if d[0] <= 0:
                deferred.remove(d)
                d[1]()
        if g < len(gsteps):
            pend, every = pend_of[gsteps[g][0]]
            if pend and gsteps[g][1] % every == every - 1:
                pend.pop(0)()
    for d in deferred:
        d[1]()
    T.barrier()
    hs.close()
    xres = sb("xres", [128, NT, D])
    for t in range(NT):
        T.dma("sp", xres[:, t, :], x_d[t * 128:(t + 1) * 128, :], w=[("xres", t)])

    def load_w_full(w_d, slots, key):
        for hf in range(2):
            T.dma("pool", wring[slots[hf]][:].rearrange("p (c n) -> p c n", n=512), wv(w_d)[:, :, hf * 512:(hf + 1) * 512],
                  w=[("wr", slots[hf])])

    def proj_residual(srcT, skeyf, slots, wkey, tiles=range(NT)):
        bi = 0
        for t in tiles:
            for hf in range(2):
                bnk = bi % 3
                bi += 1
                wsl = wring[slots[hf]][:].rearrange("p (c n) -> p c n", n=512)

                def mm(t=t, wsl=wsl, bnk=bnk):
                    for c in range(8):
                        i = PE.matmul(ps[bnk][:], lhsT=srcT[:, c, t * 128:(t + 1) * 128], rhs=wsl[:, c, :], start=(c == 0), stop=(c == 7))
                    return i
                T.op("pe", mm, r=[("wr", slots[hf])] + skeyf(t), w=[bk(bnk)])
                T.op("dve", lambda t=t, hf=hf, bnk=bnk: V.tensor_tensor(out=xres[:, t, hf * 512:(hf + 1) * 512], in0=ps[bnk][:],
                                                                       in1=xres[:, t, hf * 512:(hf + 1) * 512], op=ALU.add),
                     w=[bk(bnk), ("xres", t)])

    load_w_full(w_out_d, (0, 1), "wout")
    wout_keys = lambda t: [("mixT", H, t // 4) for H in range(8)]
    proj_residual(mixT, wout_keys, (0, 1), "wout", tiles=range(0, 8))

    hT2 = sb("hT2", [128, 8, S], BF16)
    PHB = {}

    def pre_b(t):
        def f():
            if t < 4:
                proj_residual(mixT, wout_keys, (0, 1), "wout", tiles=[8 + 2 * t, 9 + 2 * t])
            q0 = PHB.get("q0")
            if t == 4:
                q0[0]()
            if q0 is not None:
                tgq, r = divmod(t - 5, 4)
                if t >= 5 and r == 0 and tgq < 3:
                    q0[1 + 2 * tgq]()
                if t >= 7 and (t - 7) % 4 == 0 and (t - 7) // 4 < 3:
                    q0[2 + 2 * ((t - 7) // 4)]()
        return f
    PHB["pre_b"] = pre_b

    def hT2_keys(tg):
        return [("hT2", tg, c) for c in range(8)]

    crossT = mixT
    gb_list[:] = [0, 1, 2]
    rBc = xn_bufs[0][:].bitcast(F32)

    def qproj_chunks(h, ob=(6, 7)):
        slot = h % 2
        wsl = wring[slot][:, 0:2048].rearrange("p (c n) -> p c n", n=256)
        qc = qk[slot]
        chunks = []

        def load():
            T.dma("pool", wsl, wv(w_cq_d)[:, :, h * 256:(h + 1) * 256], w=[("wr", slot)])
        chunks.append(load)
        for tg in range(4):
            holder = []

            def cha(tg=tg, holder=holder):
                def mmq():
                    for b in range(2):
                        for c in range(8):
                            i = PE.matmul(ps[ob[b]][:], lhsT=wsl[:, c, b * 128:(b + 1) * 128], rhs=hT2[:, c, tg * 512:(tg + 1) * 512],
                                          start=(c == 0), stop=(c == 7))
                    return i
                T.op("pe", mmq, r=[("wr", slot)] + hT2_keys(tg), w=[bk(ob[0]), bk(ob[1])])
                emit_qknorm([ob[0], ob[1]], 256, [pp[:, 6:7], pp[:, 7:8]], [qc[:, 0, tg * 512:(tg + 1) * 512], qc[:, 1, tg * 512:(tg + 1) * 512]],
                            ["pp6"], [[("qk", slot, 0, tg)], [("qk", slot, 1, tg)]], oneb[:], n=512, defer=holder)
            chunks.append(cha)
            chunks.append(lambda holder=holder: holder.pop(0)())
        return chunks

    def cross_attention(h, pending):
        slot = h % 2
        qc = qk[slot]

        def emit_S(tg):
            ets = []
            for mb in range(2):
                sbank = next_gbank()
                etb = st_cnt[0] % 4
                st_cnt[0] += 1
                ets.append(etb)

                def mms(mb=mb, sbank=sbank):
                    for b in range(2):
                        i = PE.matmul(ps[sbank][:], lhsT=kTc[:, h, b, mb * 128:(mb + 1) * 128], rhs=qc[:, b, tg * 512:(tg + 1) * 512],
                                      start=(b == 0), stop=(b == 1))
                    return i
                T.op("pe", mms, r=[("kTc", h), ("qk", slot, 0, tg), ("qk", slot, 1, tg)], w=[bk(sbank)])
                T.op("act", lambda sbank=sbank, etb=etb: A.activation(out=ET[etb][:], in_=ps[sbank][:], func=AF.Exp), w=[bk(sbank), ("ET", etb)])
            return ets

        def emit_AV(tg, ets):
            def mma():
                for b in range(2):
                    for mb in range(2):
                        PE.matmul(ps[3 + b][:], lhsT=vc[:, mb, h, b * 128:(b + 1) * 128], rhs=ET[ets[mb]][:], start=(mb == 0), stop=(mb == 1))
                for mb in range(2):
                    i = PE.matmul(ps[5][:], lhsT=oneb[:], rhs=ET[ets[mb]][:], start=(mb == 0), stop=(mb == 1))
                return i
            T.op("pe", mma, r=[("ET", ets[0]), ("ET", ets[1]), ("vc", 0, h // 2), ("vc", 1, h // 2), "oneb"], w=[bk(3), bk(4), bk(5)])
            T.op("act", lambda: A.activation(out=rBc, in_=ps[5][:], func=AF.Ln), w=[bk(5), ("xn", 0)])
            T.op("act", lambda: A.activation(out=rBc, in_=rBc, func=AF.Exp, scale=-1.0), w=[("xn", 0)])
            for b in range(2):
                T.op("dve", lambda b=b: V.tensor_tensor(out=crossT[:, 2 * h + b, tg * 512:(tg + 1) * 512], in0=ps[3 + b][:], in1=rBc, op=ALU.mult),
                     r=[("xn", 0)], w=[bk(3 + b), ("mixT", 2 * h + b, tg)])

        return emit_S, emit_AV

    q0 = qproj_chunks(0, ob=(3, 4))
    PHB["q0"] = q0
    norm_all([(t, xres[:, t, :], [("xres", t)], PHB["pre_b"](t)) for t in range(NT)], norm_cross_g_d, hT2, "hT2")
    for ch in q0[7:]:
        ch()
    wco_keys = lambda t: [("mixT", cb, t // 4) for cb in range(8)]
    xh = {h: cross_attention(h, None) for h in range(4)}
    xpend = {h: (qproj_chunks(h + 1) if h < 3 else []) for h in range(4)}
    xsteps = [(h, tg) for h in range(4) for tg in range(4)]
    xinfos = {}
    for n in range(len(xsteps) + 1):
        if n < len(xsteps):
            h, tg = xsteps[n]
            if tg == 0 and h > 0:
                while xpend[h - 1]:
                    xpend[h - 1].pop(0)()
            if tg == 0 and h == 3:
                load_w_full(w_co_d, (0, 1), "wco")
            xinfos[n] = xh[h][0](tg)
            for _ in range(3):
                if xpend[h]:
                    xpend[h].pop(0)()
        if n >= 1:
            h2, tg2 = xsteps[n - 1]
            xh[h2][1](tg2, xinfos.pop(n - 1))
            if h2 == 3 and tg2 >= 1:
                proj_residual(crossT, wco_keys, (0, 1), "wco", tiles=range(4 * (tg2 - 1), 4 * (tg2 - 1) + 4))
    proj_residual(crossT, wco_keys, (0, 1), "wco", tiles=range(12, 16))

    T.barrier()
    norm_all([(t, xres[:, t, :], [("xres", t)], None) for t in range(NT)], norm_ffn_g_d, hT2, "hT2")
    wrt = wring[2]
    T.dma("pool", wrt[:, 0:256].rearrange("p (c n) -> p c n", n=32)[:, :, 0:4], wv(w_gr_d), w=[("wr", 2)], allow_slow_non_contiguous=True)
    T.dma("pool", wrt[:, 0:256].rearrange("p (c n) -> p c n", n=32)[:, :, 4:20], wv(w_er_d), w=[("wr", 2)], allow_slow_non_contiguous=True)

    def mmr():
        for t in range(NT):
            for c in range(8):
                i = PE.matmul(ps[0][:, 32 * t:32 * t + 20], lhsT=hT2[:, c, t * 128:(t + 1) * 128], rhs=wrt[:, 32 * c:32 * c + 20],
                              start=(c == 0), stop=(c == 7))
        return i
    T.op("pe", mmr, r=[("wr", 2)] + [k for tg in range(4) for k in hT2_keys(tg)], w=[bk(0)])
    T.op("dve", lambda: V.tensor_tensor(out=rl[:], in0=ps[0][:].rearrange("p (t n) -> p t n", n=32)[:, :, 0:20],
                                        in1=b20[:].unsqueeze(1).broadcast_to([128, NT, 20]), op=ALU.add),
         r=["b20a", "b20b"], w=[bk(0), "rl"])
    RS = rsb[0][:].rearrange("p (t n) -> p t n", n=32)
    z = rl

    def bc(ap2):
        return ap2.unsqueeze(2).broadcast_to([128, NT, 4])

    def dv(fn):
        T.op("dve", fn, r=["rl"], w=["rtr"])

    def ac(fn):
        T.op("act", fn, r=["rl"], w=["rtr"])
    gmax, ngm, sumg, pg = RS[:, :, 0], RS[:, :, 1], RS[:, :, 2], RS[:, :, 3]
    mg, eg, sel, m1, sel2, m2, gin, tmp4 = (RS[:, :, 4 + 4 * i:8 + 4 * i] for i in range(7)) if False else tuple(RS[:, :, 4 + 4 * i:8 + 4 * i] for i in range(7)) + (None,)
    v1, v2, dd, e2 = rt[:, 0:16], rt[:, 16:32], rt[:, 32:48], rt[:, 48:64]
    dv(lambda: V.tensor_reduce(out=gmax, in_=z[:, :, 0:4], axis=AX.X, op=ALU.max))
    dv(lambda: V.tensor_tensor(out=mg, in0=z[:, :, 0:4], in1=bc(gmax), op=ALU.is_equal))
    dv(lambda: V.tensor_tensor(out=eg, in0=z[:, :, 0:4], in1=bc(gmax), op=ALU.subtract))
    ac(lambda: A.activation(out=eg, in_=eg, func=AF.Exp))
    dv(lambda: V.tensor_reduce(out=sumg, in_=eg, axis=AX.X, op=ALU.add))
    dv(lambda: V.reciprocal(out=pg, in_=sumg))
    dv(lambda: V.tensor_tensor(out=sel, in0=z[:, :, 4:8], in1=bc(mg[:, :, 0]), op=ALU.mult))
    for g in range(1, 4):
        dv(lambda g=g: V.tensor_tensor(out=sel2, in0=z[:, :, 4 + 4 * g:8 + 4 * g], in1=bc(mg[:, :, g]), op=ALU.mult))
        dv(lambda: V.tensor_tensor(out=sel, in0=sel, in1=sel2, op=ALU.add))
    dv(lambda: V.tensor_reduce(out=v1, in_=sel, axis=AX.X, op=ALU.max))
    dv(lambda: V.tensor_tensor(out=m1, in0=sel, in1=bc(v1), op=ALU.is_equal))
    dv(lambda: V.scalar_tensor_tensor(out=sel2, in0=m1, scalar=-1e30, in1=sel, op0=ALU.mult, op1=ALU.add))
    dv(lambda: V.tensor_reduce(out=v2, in_=sel2, axis=AX.X, op=ALU.max))
    dv(lambda: V.tensor_tensor(out=m2, in0=sel2, in1=bc(v2), op=ALU.is_equal))
    dv(lambda: V.tensor_tensor(out=dd, in0=v2, in1=v1, op=ALU.subtract))
    ac(lambda: A.activation(out=e2, in_=dd, func=AF.Exp))
    dv(lambda: V.tensor_scalar(out=dd, in0=e2, scalar1=1.0, scalar2=None, op0=ALU.add))
    dv(lambda: V.reciprocal(out=dd, in_=dd))
    dv(lambda: V.tensor_tensor(out=v1, in0=dd, in1=pg, op=ALU.mult))
    dv(lambda: V.tensor_tensor(out=v2, in0=v1, in1=e2, op=ALU.mult))
    dv(lambda: V.tensor_tensor(out=gin, in0=m1, in1=bc(v1), op=ALU.mult))
    dv(lambda: V.tensor_tensor(out=sel2, in0=m2, in1=bc(v2), op=ALU.mult))
    dv(lambda: V.tensor_tensor(out=gin, in0=gin, in1=sel2, op=ALU.add))
    for g in range(4):
        T.op("dve", lambda g=g: V.tensor_tensor(out=gates[:, :, 4 * g:4 * g + 4], in0=gin, in1=bc(mg[:, :, g]), op=ALU.mult),
             r=["rtr"], w=[("gates", t) for t in range(NT)])

    mflat = mixT[:].rearrange("p a b -> p (a b)")
    wslots = [wring[0][:], wring[1][:], wring[2][:], mflat[:, 0:4096], mflat[:, 4096:8192], mflat[:, 8192:12288]]
    aTb = [mflat[:, 12288:14336].rearrange("p (f n) -> p f n", n=512), mflat[:, 14336:16384].rearrange("p (f n) -> p f n", n=512)]
    sil = [sqb[0], sqb[1]]

    def wslot(e, k):
        return wslots[(e % 2) * 3 + k]

    def wxk(i):
        return ("wr", i) if i < 3 else ("wx", i)

    def load_expert(e):
        s3 = (e % 2) * 3
        T.dma("pool", wslot(e, 0).rearrange("p (c n) -> p c n", n=512), wv(w_eg_d[e]), w=[wxk(s3)])
        T.dma("pool", wslot(e, 1).rearrange("p (c n) -> p c n", n=512), wv(w_eu_d[e]), w=[wxk(s3 + 1)])
        T.dma("pool", wslot(e, 2).rearrange("p (c n) -> p c n", n=1024), wv(w_ed_d[e]), w=[wxk(s3 + 2)])

    gu_cnt = [0]

    def emit_GU(e, tg):
        s3 = (e % 2) * 3
        wg = wslot(e, 0).rearrange("p (c n) -> p c n", n=512)
        wu = wslot(e, 1).rearrange("p (c n) -> p c n", n=512)
        ab = (e * 4 + tg) % 2
        for fb in range(4):
            k = gu_cnt[0] % 2
            gu_cnt[0] += 1
            gb, ub = k, 2 + k

            def mmg(fb=fb, gb=gb):
                for c in range(8):
                    i = PE.matmul(ps[gb][:], lhsT=wg[:, c, fb * 128:(fb + 1) * 128], rhs=hT2[:, c, tg * 512:(tg + 1) * 512], start=(c == 0), stop=(c == 7))
                return i

            def mmu(fb=fb, ub=ub):
                for c in range(8):
                    i = PE.matmul(ps[ub][:], lhsT=wu[:, c, fb * 128:(fb + 1) * 128], rhs=hT2[:, c, tg * 512:(tg + 1) * 512], start=(c == 0), stop=(c == 7))
                return i
            T.op("pe", mmg, r=[wxk(s3)] + hT2_keys(tg), w=[bk(gb)])
            T.op("pe", mmu, r=[wxk(s3 + 1)] + hT2_keys(tg), w=[bk(ub)])
            T.op("act", lambda gb=gb, k=k: A.activation(out=sil[k][:], in_=ps[gb][:], func=AF.Silu), w=[bk(gb), ("sil", k)])
            T.op("dve", lambda fb=fb, ub=ub, k=k: V.tensor_tensor(out=aTb[ab][:, fb, :], in0=sil[k][:], in1=ps[ub][:], op=ALU.mult),
                 r=[("sil", k)], w=[bk(ub), ("aT", ab, fb)])

    d_cnt = [0]

    def emit_D(e, tg):
        s3 = (e % 2) * 3
        wd = wslot(e, 2).rearrange("p (c n) -> p c n", n=1024)
        ab = (e * 4 + tg) % 2
        for tt in range(4):
            t = tg * 4 + tt
            for hf in range(2):
                bnk = 4 + d_cnt[0] % 3
                d_cnt[0] += 1

                def mmd(tt=tt, hf=hf, bnk=bnk):
                    for fb in range(4):
                        i = PE.matmul(ps[bnk][:], lhsT=aTb[ab][:, fb, tt * 128:(tt + 1) * 128], rhs=wd[:, fb, hf * 512:(hf + 1) * 512],
                                      start=(fb == 0), stop=(fb == 3))
                    return i
                T.op("pe", mmd, r=[wxk(s3 + 2)] + [("aT", ab, fb) for fb in range(4)], w=[bk(bnk)])
                T.op("dve", lambda t=t, hf=hf, bnk=bnk: V.scalar_tensor_tensor(out=xres[:, t, hf * 512:(hf + 1) * 512], in0=ps[bnk][:],
                                                                              scalar=gates[:, t, e:e + 1], in1=xres[:, t, hf * 512:(hf + 1) * 512],
                                                                              op0=ALU.mult, op1=ALU.add),
                     r=[("gates", t)], w=[bk(bnk), ("xres", t)])
            if e == 15:
                T.dma("sp", out_d[t * 128:(t + 1) * 128, :], xres[:, t, :], r=[("xres", t)], w=[("out", t)])

    load_expert(0)
    stepsC = [(e, tg) for e in range(16) for tg in range(4)]
    for n in range(len(stepsC) + 1):
        if n < len(stepsC):
            e, tg = stepsC[n]
            if tg == 0 and e + 1 < 16:
                pass
            emit_GU(e, tg)
            if tg == 1 and e + 1 < 16:
                load_expert(e + 1)
        if n >= 1:
            emit_D(*stepsC[n - 1])

    T.finish("sp")
    es.close()
    T.close()
    return nc, T


_CACHE = {}


def kernel(**inputs):
    if "nc" not in _CACHE:
        _CACHE["nc"] = build_program()[0]
    nc = _CACHE["nc"]
    cst = _make_consts()
    f32 = lambda a: np.ascontiguousarray(np.asarray(a, dtype=np.float32))
    shared = {"cst": cst}
    for k, v in inputs.items():
        if k in ("x", "mem"):
            continue
        a = f32(v)
        if k == "rel_bias":
            shared[k] = a
        else:
            shared[k] = np.ascontiguousarray(a[0])
    x = f32(inputs["x"])
    mem = f32(inputs["mem"])
    in_maps = []
    for b in range(8):
        m = dict(shared)
        m["x"] = np.ascontiguousarray(x[b])
        m["mem"] = np.ascontiguousarray(mem[b])
        in_maps.append(m)
    res = run_bass_kernel_spmd(nc, in_maps, core_ids=list(range(8)))
    out = np.stack([np.asarray(r["out"], dtype=np.float32) for r in res.results], axis=0)
    return out
```

```python
import math
import numpy as np
import concourse.bass as bass
import concourse.mybir as mybir
from concourse.bass_utils import run_bass_kernel_spmd

F32 = mybir.dt.float32
BF16 = mybir.dt.bfloat16
AF = mybir.ActivationFunctionType
ALU = mybir.AluOpType
AX = mybir.AxisListType

S = 2048
D = 1024
NT = 16
NEG = -30000.0
EPS = 1e-6
ENGS = ["pe", "act", "dve", "pool", "sp"]


class Tracker:
    def __init__(self, nc, n_lanes=9, n_single=88):
        self.nc = nc
        self.engs = {"pe": nc.tensor, "act": nc.scalar, "dve": nc.vector,
                     "pool": nc.gpsimd, "sp": nc.sync}
        self.NL = n_lanes + n_single
        self.n_rr = n_lanes
        self.next_single = 4 + n_lanes
        self.eidx = {e: i for i, e in enumerate(ENGS)}
        self.sems = []
        self._ctx = []
        for i in range(4 + self.NL):
            cm = nc.semaphore(f"s{i}")
            self.sems.append(cm.__enter__())
            self._ctx.append(cm)
        self.NV = 4 + self.NL
        self.cnt = np.zeros(self.NV, dtype=np.int64)
        self.cur = {e: np.zeros(self.NV, dtype=np.int64) for e in ENGS}
        self.last_w = {}
        self.readers = {}
        self.lane_rr = 0
        self.n_waits = 0
        self.n_ops = 0

    def close(self):
        for cm in reversed(self._ctx):
            cm.__exit__(None, None, None)

    def _gather_deps(self, r, w):
        deps = []
        for k in r:
            lw = self.last_w.get(k)
            if lw is not None:
                deps.append(lw)
        for k in w:
            lw = self.last_w.get(k)
            if lw is not None:
                deps.append(lw)
            rd = self.readers.get(k)
            if rd:
                deps.extend(rd.values())
        return deps

    def _emit_waits(self, eng, deps, own_slot):
        cur = self.cur[eng]
        e = self.engs[eng]
        need = {}
        for (slot, c, vc) in deps:
            if slot == own_slot and slot < 4:
                if slot == 0:
                    continue
            if cur[slot] >= c:
                continue
            if need.get(slot, (0, None))[0] < c:
                need[slot] = (c, vc)
        items = sorted(need.items(), key=lambda kv: -kv[1][0])
        for slot, (c, vc) in items:
            if cur[slot] >= c:
                continue
            val = int(c) * (16 if slot >= 4 else 1)
            e.wait_ge(self.sems[slot], val)
            self.n_waits += 1
            np.maximum(cur, vc, out=cur)
            if cur[slot] < c:
                cur[slot] = c

    def _record(self, rec, r, w):
        for k in r:
            self.readers.setdefault(k, {})[rec[0]] = rec
        for k in w:
            self.last_w[k] = rec
            self.readers[k] = {}

    def op(self, eng, fn, r=(), w=()):
        slot = self.eidx[eng]
        deps = self._gather_deps(r, w)
        self._emit_waits(eng, deps, slot)
        inst = fn()
        self.cnt[slot] += 1
        c = int(self.cnt[slot])
        inst.then_inc(self.sems[slot], 1)
        vc = self.cur[eng].copy()
        vc[slot] = c
        rec = (slot, c, vc)
        self._record(rec, r, w)
        self.n_ops += 1
        return rec

    def dma(self, q, out, in_, r=(), w=(), **kw):
        if q == "pool":
            slot = self.next_single
            self.next_single += 1
            assert slot < self.NV, "out of single-use DMA semaphores"
        else:
            lane = self.lane_rr
            self.lane_rr = (self.lane_rr + 1) % self.n_rr
            slot = 4 + lane
        deps = self._gather_deps(r, w)
        prev = int(self.cnt[slot])
        if prev > 0:
            pvc = np.zeros(self.NV, dtype=np.int64)
            pvc[slot] = prev
            deps.append((slot, prev, pvc))
        self._emit_waits(q, deps, -1)
        inst = self.engs[q].dma_start(out=out, in_=in_, **kw)
        self.cnt[slot] += 1
        c = int(self.cnt[slot])
        inst.then_inc(self.sems[slot], 16)
        vc = self.cur[q].copy()
        vc[slot] = c
        rec = (slot, c, vc)
        self._record(rec, r, w)
        self.n_ops += 1
        return rec

    def barrier(self):
        full = self.cnt.copy()
        for eng in ENGS:
            cur = self.cur[eng]
            e = self.engs[eng]
            own = self.eidx[eng]
            for slot in range(self.NV):
                if slot == own and slot < 4:
                    continue
                if cur[slot] < full[slot]:
                    val = int(full[slot]) * (16 if slot >= 4 else 1)
                    e.wait_ge(self.sems[slot], val)
                    self.n_waits += 1
                    cur[slot] = full[slot]
        self.last_w.clear()
        self.readers.clear()

    def finish(self, eng="sp"):
        cur = self.cur[eng]
        e = self.engs[eng]
        for slot in range(self.NV):
            if cur[slot] < self.cnt[slot]:
                val = int(self.cnt[slot]) * (16 if slot >= 4 else 1)
                e.wait_ge(self.sems[slot], val)
                cur[slot] = self.cnt[slot]


def _t5_bucket_np(rel):
    nb = 16
    max_exact = 8
    ret = (rel > 0).astype(np.int32) * nb
    n = np.abs(rel)
    nf = np.maximum(n, 1).astype(np.float32)
    large = max_exact + (np.log(nf / np.float32(max_exact)) / np.float32(math.log(128 / max_exact))
                         * np.float32(nb - max_exact)).astype(np.int32)
    large = np.minimum(large, nb - 1)
    return ret + np.where(n < max_exact, n, large)


C_ID, C_BLK, C_ONE, C_TRI, C_SEL, C_MD, C_MF, C_G = 0, 128, 256, 384, 512, 640, 768, 896
C_W = 896 + 384


def _make_consts():
    c = np.zeros((128, C_W), dtype=np.float32)
    p = np.arange(128)[:, None]
    f = np.arange(128)[None, :]
    c[:, C_ID:C_ID + 128] = (p == f)
    c[:, C_BLK:C_BLK + 128] = ((p // 64) == (f // 64))
    c[:, C_ONE:C_ONE + 128] = 1.0
    c[:, C_TRI:C_TRI + 128] = (p <= f)
    c[:, C_SEL:C_SEL + 128] = (p == 127)
    c[:, C_MD:C_MD + 128] = np.where((p >= 64) & (f < 64), NEG, 0.0)
    c[:, C_MF:C_MF + 128] = np.where(p <= f, 0.0, NEG)
    u = np.arange(383)
    rel = u - 255
    bk = _t5_bucket_np(rel.astype(np.int32))
    g = np.zeros((32, 384), dtype=np.float32)
    g[bk, u] = 1.0
    c[0:32, C_G:C_G + 384] = g
    return c


def build_program():
    nc = bass.Bass("TRN2", target_bir_lowering=False)

    def din(name, shape):
        return nc.dram_tensor(name, list(shape), F32, kind="ExternalInput").ap()

    x_d = din("x", [S, D])
    mem_d = din("mem", [256, D])
    rel_bias_d = din("rel_bias", [32, 4])
    norm_mix_g_d = din("norm_mix_g", [D])
    w_in_d = din("w_in", [D, 3076])
    b_forget_d = din("b_forget", [4])
    dqg_d = din("diff_q_norm_g", [64])
    dkg_d = din("diff_k_norm_g", [64])
    lq1_d = din("diff_lambda_q1", [64])
    lk1_d = din("diff_lambda_k1", [64])
    lq2_d = din("diff_lambda_q2", [64])
    lk2_d = din("diff_lambda_k2", [64])
    dsub_d = din("diff_subln_g", [128])
    fqg_d = din("fox_q_norm_g", [128])
    fkg_d = din("fox_k_norm_g", [128])
    fog_d = din("fox_out_norm_g", [128])
    w_out_d = din("w_out", [D, D])
    norm_cross_g_d = din("norm_cross_g", [D])
    norm_mem_g_d = din("norm_mem_g", [D])
    w_cq_d = din("w_cq", [D, D])
    w_ckv_d = din("w_ckv", [D, 2 * D])
    cqg_d = din("cross_q_norm_g", [256])
    ckg_d = din("cross_k_norm_g", [256])
    w_co_d = din("w_co", [D, D])
    norm_ffn_g_d = din("norm_ffn_g", [D])
    w_gr_d = din("w_group_router", [D, 4])
    b_gr_d = din("b_group_router", [4])
    w_er_d = din("w_expert_router", [D, 16])
    b_er_d = din("b_expert_router", [16])
    w_eg_d = din("w_exp_gate", [16, D, 512])
    w_eu_d = din("w_exp_up", [16, D, 512])
    w_ed_d = din("w_exp_down", [16, 512, D])
    cst_d = din("cst", [128, C_W])
    out_d = nc.dram_tensor("out", [S, D], F32, kind="ExternalOutput").ap()

    T = Tracker(nc)
    V, A, PE = nc.vector, nc.scalar, nc.tensor
    LAM_INIT = 0.8 - 0.6 * math.exp(0.0)

    def wv(ap):
        return ap.rearrange("(c p) n -> p c n", p=128)

    from contextlib import ExitStack
    es = ExitStack()

    def sb(name, shape, dt=F32):
        return es.enter_context(nc.sbuf_tensor(name, list(shape), dt))

    cst = sb("cst_sb", [128, C_W])
    identb = sb("identb", [128, 128], BF16)
    blkb = sb("blkb", [128, 128], BF16)
    oneb = sb("oneb", [128, 128], BF16)
    gn = sb("gn", [128, 4, 8])
    pp = sb("pp", [128, 16])
    lams = sb("lams", [128, 8])
    b20 = sb("b20", [128, 20])
    sqb = [sb(f"sq{i}", [128, 512], BF16) for i in range(2)]
    rsb = [sb(f"rs{i}", [128, 512]) for i in range(2)]
    ET = [sb(f"ET{i}", [128, 512], BF16) for i in range(6)]
    qk = [sb(f"qk{i}", [128, 2, S], BF16) for i in range(2)]
    obn = sb("obn", [128, 4, 128], BF16)
    sm = sb("sm", [128, 32])
    wring = [sb(f"wr{i}", [128, 4096], BF16) for i in range(3)]
    rl = sb("rl", [128, 16, 20])
    gates = sb("gates", [128, 16, 16])
    rt = sb("rt", [128, 64])
    xn_bufs = [sb(f"xn{i}", [128, D], BF16) for i in range(2)]
    junk = sb("junk", [128, D], BF16)
    kTc = sb("kTc", [128, 4, 2, 256], BF16)
    vc = sb("vc", [128, 2, 4, 256], BF16)
    mixT = sb("mixT", [128, 8, S], BF16)
    hs = ExitStack()

    def sba(name, shape, dt=F32):
        return hs.enter_context(nc.sbuf_tensor(name, list(shape), dt))
    lamw = sba("lamw", [128, 4, 64])
    bfo = sba("bfo", [128, 4])
    rb = sba("rb", [32, 4])
    c15 = sba("c15", [128, 4])
    BT = sba("BT", [128, 4, 5, 128])
    fl = sba("fl", [128, 16, 4])
    cumL = sba("cumL", [128, 16, 4])
    carry = sba("carry", [128, 16, 4])
    cref = sba("cref", [128, 16, 4])
    fbt = sba("fbt", [128, 4, 16, 16])
    tmpn = [sba(f"tmpn{i}", [128, 512]) for i in range(2)]
    vb = [sba(f"vb{i}", [128, 16, 128], BF16) for i in range(2)]
    qz = [sba(f"qz{i}", [128, S], BF16) for i in range(2)]
    o1 = sba("o1", [128, 4, 128])
    ob = sba("ob", [128, 4, 128])
    rB = sba("rB", [128, 512])
    mf2 = sba("mf2", [128, 256])
    obB = sba("obB", [128, 4, 128])
    obnB = sba("obnB", [128, 4, 128], BF16)
    rB2 = [sba(f"rB2_{i}", [128, 512]) for i in range(2)]
    memT = sba("memT", [128, 8, 256], BF16)
    hT = sba("hT", [128, 8, S], BF16)
    xs = [sba(f"xs{i}", [128, D], F32) for i in range(2)]

    ps = [es.enter_context(nc.psum_tensor(f"ps{i}", [128, 512], F32)) for i in range(8)]
    psT = ps[7][:].bitcast(BF16)
    gb_cnt = [0]

    gb_list = [0, 1, 2, 6, 7]

    def next_gbank():
        b = gb_list[gb_cnt[0] % len(gb_list)]
        gb_cnt[0] += 1
        return b

    def bk(i):
        return ("bank", i)

    T.dma("sp", cst[:], cst_d[:, :], w=["cst"])
    T.op("dve", lambda: V.tensor_copy(out=identb[:], in_=cst[:, C_ID:C_ID + 128]), r=["cst"], w=["identb"])
    T.op("dve", lambda: V.tensor_copy(out=blkb[:], in_=cst[:, C_BLK:C_BLK + 128]), r=["cst"], w=["blkb"])
    T.op("dve", lambda: V.tensor_copy(out=oneb[:], in_=cst[:, C_ONE:C_ONE + 128]), r=["cst"], w=["oneb"])
    bt_chunks = []
    for t in range(2):
        for f0 in range(0, 128, 16):
            def mmb(t=t, f0=f0):
                if t == 1 and f0 >= 96:
                    for h in range(4):
                        T.op("dve", lambda h=h: V.tensor_copy(out=BT[:, h, 1, f0:f0 + 16], in_=c15[:, h:h + 1].broadcast_to([128, 16])),
                             r=["c15"], w=[("BT", h)])
                    return
                gbk = next_gbank()

                def mm():
                    for f in range(f0, f0 + 16):
                        u0 = 255 - f - 128 * t
                        i = PE.matmul(ps[gbk][:, 4 * (f - f0):4 * (f - f0) + 4], lhsT=cst[0:32, C_G + u0:C_G + u0 + 128],
                                      rhs=rb[:, :], start=True, stop=True)
                    return i
                T.op("pe", mm, r=["cst", "rb"], w=[bk(gbk)])
                for h in range(4):
                    src = ps[gbk][:, 0:64].rearrange("p (f h) -> p f h", h=4)[:, :, h]
                    T.op("dve", lambda h=h, src=src: V.tensor_copy(out=BT[:, h, t, f0:f0 + 16], in_=src), w=[bk(gbk), ("BT", h)])
            bt_chunks.append(mmb)

    def bt_finish():
        for h in range(4):
            T.op("dve", lambda h=h: V.tensor_tensor(out=BT[:, h, 0, :], in0=BT[:, h, 0, :], in1=cst[:, C_MD:C_MD + 128], op=ALU.add),
                 r=["cst"], w=[("BT", h)])
            T.op("dve", lambda h=h: V.tensor_copy(out=BT[:, h, 2:5, :].rearrange("p t q -> p (t q)"),
                                                  in_=c15[:, h:h + 1].broadcast_to([128, 384])),
                 r=["c15"], w=[("BT", h)])

    def rstd_small(src_ss, n, scale, keyr, dst, keyw):
        T.op("act", lambda: A.activation(out=dst, in_=src_ss, func=AF.Ln, scale=scale, bias=EPS), r=[keyr], w=[keyw])
        T.op("act", lambda: A.activation(out=dst, in_=dst, func=AF.Exp, scale=-0.5), r=[keyw], w=[keyw])

    nrm_cnt = [0]

    gB = qk[1][:, 0, :].bitcast(F32)
    GBK = [("qk", 1, 0, tg) for tg in range(4)]

    def norm_stage1(src_ap, src_keys, n):
        b = n % 2
        sc = sm[:, 16 + 2 * b:16 + 2 * b + 1]
        sr = sm[:, 17 + 2 * b:17 + 2 * b + 1]
        T.op("act", lambda: A.activation(out=junk[:], in_=src_ap, func=AF.Square, accum_out=sc), r=src_keys, w=["junk", ("nss", b)])
        rstd_small(sc, 1, 1.0 / D, ("nss", b), sr, ("nrs", b))
        T.op("dve", lambda: V.scalar_tensor_tensor(out=xn_bufs[b][:], in0=src_ap, scalar=sr, in1=gB, op0=ALU.mult, op1=ALU.mult),
             r=list(src_keys) + [("nrs", b)] + GBK, w=[("xn", b)])

    def norm_stage2(n, dstT, dkey, t):
        b = n % 2
        nb_ = 6 + b
        pT = ps[nb_][:].bitcast(BF16)

        def tr():
            for c in range(8):
                i = PE.transpose(out=pT[:, c * 128:(c + 1) * 128], in_=xn_bufs[b][:, c * 128:(c + 1) * 128], identity=identb[:])
            return i
        T.op("pe", tr, r=[("xn", b), "identb"], w=[bk(nb_)])
        T.op("dve", lambda: V.tensor_copy(out=dstT[:, :, t * 128:(t + 1) * 128], in_=pT.rearrange("p (c n) -> p c n", n=128)),
             w=[bk(nb_)] + [(dkey, t // 4, c) for c in range(8)])

    def norm_all(items, g_d, dstT, dkey):
        T.dma("sp", gB, g_d.partition_broadcast(128), w=GBK)
        prev = None
        for (t, src_ap, src_keys, pre) in items:
            if pre is not None:
                pre()
            n = nrm_cnt[0]
            nrm_cnt[0] += 1
            norm_stage1(src_ap, src_keys, n)
            if prev is not None:
                norm_stage2(prev[0], dstT, dkey, prev[1])
            prev = (n, t)
        norm_stage2(prev[0], dstT, dkey, prev[1])

    qkn_cnt = [0]

    def emit_qknorm(banks, dk, gcols, dsts, rkeys, wkeys, blk_ap, n=512, ssbank=None, defer=None, split=None):
        i0 = qkn_cnt[0]
        qkn_cnt[0] += 1
        sqs = []
        for bi, bnk in enumerate(banks):
            sq = sqb[i0 % 2] if len(banks) == 1 else sqb[bi]
            sqs.append(sq)
            T.op("act", lambda bnk=bnk, sq=sq: A.activation(out=sq[:, 0:n], in_=ps[bnk][:, 0:n], func=AF.Square),
                 w=[bk(bnk), ("sq", id(sq))])

        def part_b():
            _qknorm_b(banks, dk, gcols, dsts, rkeys, wkeys, blk_ap, n, (next_gbank() if ssbank is None else ssbank), sqs, i0, split)
        if defer is not None:
            defer.append(part_b)
        else:
            part_b()

    def _qknorm_b(banks, dk, gcols, dsts, rkeys, wkeys, blk_ap, n, ssbank, sqs, i0, split):
        def mss():
            for bi, sq in enumerate(sqs):
                i = PE.matmul(ps[ssbank][:, 0:n], lhsT=blk_ap, rhs=sq[:, 0:n], start=(bi == 0), stop=(bi == len(sqs) - 1))
            return i
        T.op("pe", mss, r=[("sq", id(sq)) for sq in sqs] + ["blkb", "oneb"], w=[bk(ssbank)])
        rs = rsb[i0 % 2]
        T.op("act", lambda: A.activation(out=rs[:, 0:n], in_=ps[ssbank][:, 0:n], func=AF.Ln, scale=1.0 / dk, bias=EPS),
             w=[bk(ssbank), ("rs", i0 % 2)])
        T.op("act", lambda: A.activation(out=rs[:, 0:n], in_=rs[:, 0:n], func=AF.Exp, scale=-0.5), w=[("rs", i0 % 2)])
        if split is not None:
            bnk = banks[0]
            for (lo, hi, dst, wk) in split:
                T.op("dve", lambda lo=lo, hi=hi, dst=dst: V.scalar_tensor_tensor(out=dst, in0=ps[bnk][lo:hi, 0:n], scalar=gcols[0][lo:hi],
                                                                                  in1=rs[lo:hi, 0:n], op0=ALU.mult, op1=ALU.mult),
                     r=[("rs", i0 % 2)] + list(rkeys), w=[bk(bnk)] + list(wk))
            return
        for bi, bnk in enumerate(banks):
            T.op("dve", lambda bi=bi, bnk=bnk: V.scalar_tensor_tensor(out=dsts[bi], in0=ps[bnk][:, 0:n], scalar=gcols[bi],
                                                                       in1=rs[:, 0:n], op0=ALU.mult, op1=ALU.mult),
                 r=[("rs", i0 % 2)] + list(rkeys), w=[bk(bnk)] + list(wkeys[bi]))

    def pre_a(t):
        def f():
            T.dma("sp", xs[t % 2][:], x_d[t * 128:(t + 1) * 128, :], w=[("xs", t % 2)])
        return f
    norm_all([(t, xs[t % 2][:], [("xs", t % 2)], pre_a(t)) for t in range(NT)], norm_mix_g_d, hT, "hT")

    wq_early = wring[0][:, 0:3072].rearrange("p (c n) -> p c n", n=384)
    for i_, c0_ in enumerate((1536, 2048, 2560)):
        T.dma("pool", wq_early[:, :, i_ * 128:(i_ + 1) * 128], wv(w_in_d)[:, :, c0_:c0_ + 128], w=[("wr", 0)])
    def col(ap):
        return ap.rearrange("(p o) -> p o", o=1)
    T.dma("sp", pp[0:64, 0:1], col(dqg_d), w=["pp0"], allow_slow_non_contiguous=True)
    T.dma("sp", pp[64:128, 0:1], col(dqg_d), w=["pp0"], allow_slow_non_contiguous=True)
    T.dma("sp", pp[0:64, 1:2], col(dkg_d), w=["pp1"], allow_slow_non_contiguous=True)
    T.dma("sp", pp[64:128, 1:2], col(dkg_d), w=["pp1"], allow_slow_non_contiguous=True)
    T.dma("sp", pp[:, 2:3], col(fqg_d), w=["pp2"], allow_slow_non_contiguous=True)
    T.dma("sp", pp[:, 3:4], col(fkg_d), w=["pp3"], allow_slow_non_contiguous=True)
    T.dma("sp", pp[:, 4:5], col(dsub_d), w=["pp4"], allow_slow_non_contiguous=True)
    T.dma("sp", pp[:, 5:6], col(fog_d), w=["pp5"], allow_slow_non_contiguous=True)
    T.dma("sp", pp[:, 6:8], cqg_d.rearrange("(c p) -> p c", p=128), w=["pp6"], allow_slow_non_contiguous=True)
    T.dma("sp", pp[:, 8:10], ckg_d.rearrange("(c p) -> p c", p=128), w=["pp8"], allow_slow_non_contiguous=True)
    T.op("dve", lambda: V.tensor_scalar(out=pp[:, 0:1], in0=pp[:, 0:1], scalar1=64 ** -0.5, scalar2=None, op0=ALU.mult), r=["pp0"], w=["pp0"])
    T.op("dve", lambda: V.tensor_scalar(out=pp[:, 2:3], in0=pp[:, 2:3], scalar1=128 ** -0.5, scalar2=None, op0=ALU.mult), r=["pp2"], w=["pp2"])
    T.op("dve", lambda: V.tensor_scalar(out=pp[:, 4:5], in0=pp[:, 4:5], scalar1=1.0 - LAM_INIT, scalar2=None, op0=ALU.mult), r=["pp4"], w=["pp4"])
    T.op("dve", lambda: V.tensor_scalar(out=pp[:, 6:8], in0=pp[:, 6:8], scalar1=256 ** -0.5, scalar2=None, op0=ALU.mult), r=["pp6"], w=["pp6"])
    for i, l_d in enumerate([lq1_d, lk1_d, lq2_d, lk2_d]):
        T.dma("sp", lamw[:, i, :], l_d.partition_broadcast(128), w=[("lamw", i)])
    T.op("dve", lambda: V.tensor_tensor(out=lamw[:, 0, :], in0=lamw[:, 0, :], in1=lamw[:, 1, :], op=ALU.mult), r=[("lamw", 1)], w=[("lamw", 0)])
    T.op("dve", lambda: V.tensor_tensor(out=lamw[:, 2, :], in0=lamw[:, 2, :], in1=lamw[:, 3, :], op=ALU.mult), r=[("lamw", 3)], w=[("lamw", 2)])
    T.op("dve", lambda: V.reduce_sum(out=lams[:, 0:1], in_=lamw[:, 0, :], axis=AX.X), r=[("lamw", 0)], w=["lams0"])
    T.op("dve", lambda: V.reduce_sum(out=lams[:, 1:2], in_=lamw[:, 2, :], axis=AX.X), r=[("lamw", 2)], w=["lams1"])
    T.op("act", lambda: A.activation(out=lams[:, 2:4], in_=lams[:, 0:2], func=AF.Exp), r=["lams0", "lams1"], w=["lams2"])
    T.op("dve", lambda: V.tensor_tensor(out=lams[:, 4:5], in0=lams[:, 2:3], in1=lams[:, 3:4], op=ALU.subtract), r=["lams2"], w=["lams4"])
    T.op("dve", lambda: V.tensor_scalar(out=lams[:, 5:6], in0=lams[:, 4:5], scalar1=-1.0, scalar2=-LAM_INIT, op0=ALU.mult, op1=ALU.add), r=["lams4"], w=["neglam"])
    T.dma("sp", bfo[:], b_forget_d.partition_broadcast(128), w=["bfo"])
    T.dma("sp", b20[:, 0:4], b_gr_d.partition_broadcast(128), w=["b20a"])
    T.dma("sp", b20[:, 4:20], b_er_d.partition_broadcast(128), w=["b20b"])
    T.dma("sp", rb[:], rel_bias_d[:, :], w=["rb"])
    T.dma("sp", c15[:], rel_bias_d[15:16, :].partition_broadcast(128).rearrange("p o h -> p (o h)"), w=["c15"])


    def hT_keys(tg):
        return [("hT", tg, c) for c in range(8)]

    wf = wring[2]
    T.dma("pool", wf[:, 0:32].rearrange("p (c n) -> p c n", n=4), wv(w_in_d)[:, :, 3072:3076], w=[("wr", 2)],
          allow_slow_non_contiguous=True)

    def mmf():
        for t in range(NT):
            for c in range(8):
                i = PE.matmul(ps[5][:, 4 * t:4 * t + 4], lhsT=hT[:, c, t * 128:(t + 1) * 128], rhs=wf[:, 4 * c:4 * c + 4],
                              start=(c == 0), stop=(c == 7))
        return i
    T.op("pe", mmf, r=[("wr", 2)] + [k for tg in range(4) for k in hT_keys(tg)], w=[bk(5)])
    for h in range(4):
        src = ps[5][:, 0:64].rearrange("p (t h) -> p t h", h=4)[:, :, h]
        T.op("dve", lambda h=h, src=src: V.tensor_scalar(out=fl[:, :, h], in0=src, scalar1=bfo[:, h:h + 1], scalar2=None, op0=ALU.add),
             r=["bfo"], w=[bk(5), "fl"])
    flf = fl[:].rearrange("p t h -> p (t h)")
    T.op("act", lambda: A.activation(out=flf, in_=flf, func=AF.Exp, scale=-1.0), w=["fl"])
    T.op("act", lambda: A.activation(out=flf, in_=flf, func=AF.Ln, bias=1.0), w=["fl"])
    T.op("pe", lambda: PE.matmul(ps[5][:, 0:64], lhsT=cst[:, C_TRI:C_TRI + 128], rhs=flf, start=True, stop=True),
         r=["fl", "cst"], w=[bk(5)])
    T.op("pe", lambda: PE.matmul(ps[6][:, 0:64], lhsT=cst[:, C_ONE:C_ONE + 128], rhs=flf, start=True, stop=True),
         r=["fl", "cst"], w=[bk(6)])
    T.op("dve", lambda: V.memset(carry[:, 0, :], 0.0), w=["carry"])
    totv = ps[6][:, 0:64].rearrange("p (t h) -> p t h", h=4)
    for j in range(1, NT):
        T.op("dve", lambda j=j: V.tensor_tensor(out=carry[:, j, :], in0=carry[:, j - 1, :], in1=totv[:, j - 1, :], op=ALU.add),
             w=["carry", bk(6)])
    T.op("dve", lambda: V.tensor_tensor(out=cumL[:].rearrange("p t h -> p (t h)"), in0=ps[5][:, 0:64],
                                        in1=carry[:].rearrange("p t h -> p (t h)"), op=ALU.add), r=["carry"], w=[bk(5), "cumL"])
    T.op("pe", lambda: PE.matmul(ps[5][:, 0:64], lhsT=cst[:, C_SEL:C_SEL + 128], rhs=cumL[:].rearrange("p t h -> p (t h)"),
                                 start=True, stop=True), r=["cumL", "cst"], w=[bk(5)])
    T.op("dve", lambda: V.tensor_scalar(out=cref[:].rearrange("p t h -> p (t h)"), in0=ps[5][:, 0:64], scalar1=-1.0, scalar2=None,
                                        op0=ALU.mult), w=[bk(5), "cref"])
    for h in range(4):
        T.op("dve", lambda h=h: V.tensor_tensor(out=fbt[:, h, :, :], in0=cumL[:, :, h].unsqueeze(2).broadcast_to([128, NT, NT]),
                                                in1=cref[:, :, h].unsqueeze(1).broadcast_to([128, NT, NT]), op=ALU.add),
             r=["cref", "cumL"], w=[("fbt", h)])

    def pre_m(mt):
        return lambda: T.dma("sp", xs[mt][:], mem_d[mt * 128:(mt + 1) * 128, :], w=[("xs", mt)])
    norm_all([(mt, xs[mt][:], [("xs", mt)], pre_m(mt)) for mt in range(2)], norm_mem_g_d, memT, "memT")
    memkeys = [("memT", 0, c) for c in range(8)]
    def mem_piece(piece):
        T.dma("pool", wring[2][:].rearrange("p (c n) -> p c n", n=512), wv(w_ckv_d)[:, :, piece * 512:(piece + 1) * 512], w=[("wr", 2)])
        wsl = wring[2][:].rearrange("p (c n) -> p c n", n=512)
        if piece < 2:
            for hh in range(2):
                h = piece * 2 + hh

                def mmk(hh=hh):
                    for b in range(2):
                        for c in range(8):
                            i = PE.matmul(ps[b][:, 0:256], lhsT=wsl[:, c, hh * 256 + b * 128:hh * 256 + (b + 1) * 128], rhs=memT[:, c, :],
                                          start=(c == 0), stop=(c == 7))
                    return i
                T.op("pe", mmk, r=[("wr", 2)] + memkeys, w=[bk(0), bk(1)])
                emit_qknorm([0, 1], 256, [pp[:, 8:9], pp[:, 9:10]], [kTc[:, h, 0, :], kTc[:, h, 1, :]], ["pp8"],
                            [[("kTc", h)], [("kTc", h)]], oneb[:], n=256, ssbank=2)
        else:
            half = piece - 2
            for mt in range(2):
                def mmv(mt=mt):
                    for c in range(8):
                        i = PE.matmul(ps[mt][:], lhsT=memT[:, c, mt * 128:(mt + 1) * 128], rhs=wsl[:, c, :], start=(c == 0), stop=(c == 7))
                    return i
                T.op("pe", mmv, r=[("wr", 2)] + memkeys, w=[bk(mt)])
                T.op("dve", lambda mt=mt, half=half: V.tensor_copy(out=vc[:, mt, 2 * half:2 * half + 2, :],
                                                                  in_=ps[mt][:].rearrange("p (a b) -> p a b", b=256)),
                     w=[bk(mt), ("vc", mt, half)])
    mem_chunks = [(lambda p=p: mem_piece(p)) for p in range(4)]
    T.op("dve", lambda: V.tensor_copy(out=mf2[:, 0:128], in_=cst[:, C_MF:C_MF + 128]), r=["cst"], w=["mf2"])
    T.op("pool", lambda: nc.gpsimd.memset(mf2[:, 128:256], 0.0), w=["mf2"])

    for i in range(2):
        T.op("pool", lambda i=i: nc.gpsimd.memset(qk[i][64:128, 0, :], 0.0), w=[("qk", i, 0, tg) for tg in range(4)])
        T.op("pool", lambda i=i: nc.gpsimd.memset(qz[i][0:64, :], 0.0), w=[("qz", i, tg) for tg in range(4)])


    def head_cols(H):
        if H < 4:
            return (H * 128, 512 + H * 128, 1024 + H * 128)
        h = H - 4
        return (1536 + h * 128, 2048 + h * 128, 2560 + h * 128)

    def proj_chunks(H, pbanks=(5,)):
        slot = H % 2
        wsl = wring[slot]
        cq, ck, cv = head_cols(H)
        wq = wsl[:, 0:3072].rearrange("p (c n) -> p c n", n=384)
        chunks = []

        isdiff = H < 4

        def load():
            for i, c0 in enumerate((cq, ck, cv)):
                T.dma("pool", wq[:, :, i * 128:(i + 1) * 128], wv(w_in_d)[:, :, c0:c0 + 128], w=[("wr", slot)])
            if isdiff:
                T.op("pool", lambda: nc.gpsimd.memset(qk[slot][64:128, 0, :], 0.0), w=[("qk", slot, 0, tg) for tg in range(4)])
        chunks.append(load)
        dk = 64 if isdiff else 128
        blk_ap = blkb[:] if isdiff else oneb[:]
        for which in range(2):
            gcol = pp[:, (0 if isdiff else 2) + which:(0 if isdiff else 2) + which + 1]
            gkey = f"pp{(0 if isdiff else 2) + which}"
            for tg in range(4):
                pb = pbanks[(which * 4 + tg) % len(pbanks)]

                def ch(which=which, tg=tg, gcol=gcol, gkey=gkey, holder=None, pb=pb):
                    def mm():
                        for c in range(8):
                            i = PE.matmul(ps[pb][:], lhsT=wq[:, c, which * 128:(which + 1) * 128], rhs=hT[:, c, tg * 512:(tg + 1) * 512],
                                          start=(c == 0), stop=(c == 7))
                        return i
                    T.op("pe", mm, r=[("wr", slot)] + hT_keys(tg), w=[bk(pb)])
                    cs_ = slice(tg * 512, (tg + 1) * 512)
                    sp = None
                    if isdiff and which == 0:
                        sp = [(0, 64, qk[slot][0:64, 0, cs_], [("qk", slot, 0, tg)]),
                              (64, 128, qz[slot][64:128, cs_], [("qz", slot, tg)])]
                    emit_qknorm([pb], dk, [gcol], [qk[slot][:, which, cs_]], [gkey],
                                [[("qk", slot, which, tg)]], blk_ap, defer=holder, split=sp)
                holder = []
                ch.__defaults__ = ch.__defaults__
                chunks.append((lambda ch=ch, holder=holder: ch(holder=holder)))
                chunks.append((lambda holder=holder: holder.pop(0)()))
        for tg in range(4):
            def chv(tg=tg):
                def mm():
                    for tt in range(4):
                        t = tg * 4 + tt
                        for c in range(8):
                            i = PE.matmul(ps[5][:, tt * 128:(tt + 1) * 128], lhsT=hT[:, c, t * 128:(t + 1) * 128],
                                          rhs=wq[:, c, 256:384], start=(c == 0), stop=(c == 7))
                    return i
                T.op("pe", mm, r=[("wr", slot)] + hT_keys(tg), w=[bk(5)])
                T.op("dve", lambda: V.tensor_copy(out=vb[slot][:, tg * 4:(tg + 1) * 4, :],
                                                  in_=ps[5][:].rearrange("p (a b) -> p a b", b=128)),
                     w=[bk(5), ("vb", slot, tg)])
            chunks.append(chv)
        return chunks

    st_cnt = [0]
    fin_cnt = [0]

    def attention(H, pending, dstT):
        slot = H % 2
        isdiff = H < 4
        h = H if isdiff else H - 4
        qT = qk[slot][:, 0, :]
        kT = qk[slot][:, 1, :]
        vv = vb[slot]
        comps = [0, 1] if isdiff else [0]
        steps = [(I, c, j) for I in range(4) for c in comps for j in range(4 * I + 4)]

        def acc_ap(il):
            return ps[3 + il // 2][:, (il % 2) * 256:(il % 2) * 256 + 129]

        def emit_S(n):
            I, c, j = steps[n]
            i0 = max(4 * I, j)
            ncol = (4 * I + 4 - i0) * 128
            q0 = i0 * 128
            sbank = next_gbank()
            etb = st_cnt[0] % 6
            st_cnt[0] += 1
            et = ET[etb]
            if isdiff and c == 1:
                rk = [("qz", slot, I), ("qk", slot, 1, j // 4)]
                qsrc = qz[slot]
            else:
                rk = [("qk", slot, 0, I), ("qk", slot, 1, j // 4)]
                qsrc = qT
            T.op("pe", lambda: PE.matmul(ps[sbank][:, 0:ncol], lhsT=kT[:, j * 128:(j + 1) * 128], rhs=qsrc[:, q0:q0 + ncol],
                                         start=True, stop=True), r=rk, w=[bk(sbank)])
            nb = ncol // 128
            late = []
            if isdiff:
                t_lo = i0 - j
                n_near = 0 if t_lo >= 2 else min(nb, 2 - t_lo)
                if n_near > 0:
                    tb = tmpn[n % 2]
                    tkey = ("tmpn", n % 2)
                    bsrc = BT[:, h, t_lo:t_lo + nb, :].rearrange("p t q -> p (t q)")
                    T.op("dve", lambda: V.tensor_tensor(out=tb[:, 0:ncol], in0=ps[sbank][:, 0:ncol], in1=bsrc, op=ALU.add),
                         r=[("BT", h)], w=[bk(sbank), tkey])
                    late.append((lambda: A.activation(out=et[:, 0:ncol], in_=tb[:, 0:ncol], func=AF.Exp), [tkey]))
                else:
                    T.op("act", lambda: A.activation(out=et[:, 0:ncol], in_=ps[sbank][:, 0:ncol], func=AF.Exp, bias=c15[:, h:h + 1]),
                         r=["c15"], w=[bk(sbank), ("ET", etb)])
            else:
                runs = []
                merged_pair = [None]
                for bi in range(nb):
                    i = i0 + bi
                    pr = i // 2
                    if i == j:
                        wd = 256 if (i % 2 == 0 and bi + 1 < nb) else 128
                        if wd == 256:
                            merged_pair[0] = pr
                        cs = slice(bi * 128, bi * 128 + wd)
                        tb = tmpn[n % 2]
                        tkey = ("tmpn", n % 2)
                        T.op("dve", lambda cs=cs, tb=tb, wd=wd: V.tensor_tensor(out=tb[:, 0:wd], in0=ps[sbank][:, cs], in1=mf2[:, 0:wd], op=ALU.add),
                             r=["mf2"], w=[bk(sbank), tkey])
                        late.append((lambda cs=cs, tb=tb, pr=pr, wd=wd: A.activation(out=et[:, cs], in_=tb[:, 0:wd], func=AF.Exp,
                                                                                      bias=fbt[:, h, j, 2 * pr:2 * pr + 1]),
                                     [tkey, ("fbt", h)]))
                    elif merged_pair[0] == pr:
                        continue
                    elif runs and runs[-1][2] == pr and runs[-1][1] == bi * 128:
                        runs[-1] = (runs[-1][0], (bi + 1) * 128, pr)
                    else:
                        runs.append((bi * 128, (bi + 1) * 128, pr))
                for (lo_, hi_, pr) in runs:
                    T.op("act", lambda lo_=lo_, hi_=hi_, pr=pr: A.activation(out=et[:, lo_:hi_], in_=ps[sbank][:, lo_:hi_], func=AF.Exp,
                                                                            bias=fbt[:, h, j, 2 * pr:2 * pr + 1]),
                         r=[("fbt", h)], w=[bk(sbank), ("ET", etb)])
            for fn, rk_ in late:
                T.op("act", fn, r=rk_, w=[("ET", etb)])
            return (etb, i0, nb)

        def accb(I, c):
            return (3, 4)

        def emit_AV(n, info):
            I, c, j = steps[n]
            etb, i0, nb = info
            et = ET[etb]
            ncol = nb * 128
            c0 = (i0 - 4 * I) * 128
            first = (j == 0)
            last = (j == 4 * I + 3)

            bA, bB = accb(I, c)

            def mm():
                PE.matmul(ps[bA][:, c0:c0 + ncol], lhsT=vv[:, j, :], rhs=et[:, 0:ncol], start=first, stop=last)
                return PE.matmul(ps[bB][:, c0:c0 + ncol], lhsT=oneb[:], rhs=et[:, 0:ncol], start=first, stop=last)
            T.op("pe", mm, r=[("ET", etb), ("vb", slot, j // 4), "oneb"], w=[bk(bA), bk(bB)])
            if last:
                finalize(I, c)

        o1f = o1[:].rearrange("p a b -> p (a b)")
        obfs = [ob[:].rearrange("p a b -> p (a b)"), obB[:].rearrange("p a b -> p (a b)")]
        obnfs = [obn[:].rearrange("p a b -> p (a b)"), obnB[:].rearrange("p a b -> p (a b)")]

        def finalize(I, c):
            bA, bB = accb(I, c)
            T.op("act", lambda: A.activation(out=rB[:], in_=ps[bB][:], func=AF.Ln), w=[bk(bB), "rB"])
            T.op("act", lambda: A.activation(out=rB[:], in_=rB[:], func=AF.Exp, scale=-1.0), w=["rB"])
            if isdiff and c == 0:
                T.op("dve", lambda: V.tensor_tensor(out=o1f, in0=ps[bA][:], in1=rB[:], op=ALU.mult), r=["rB"], w=[bk(bA), "o1"])
                return
            par = fin_cnt[0] % 2
            fin_cnt[0] += 1
            obf, obnf, rr = obfs[par], obnfs[par], rB2[par]
            T.op("dve", lambda: V.tensor_tensor(out=obf, in0=ps[bA][:], in1=rB[:], op=ALU.mult), r=["rB"], w=[bk(bA), ("ob", par)])
            if isdiff:
                T.op("dve", lambda: V.scalar_tensor_tensor(out=obf, in0=obf, scalar=lams[:, 5:6], in1=o1f, op0=ALU.mult, op1=ALU.add),
                     r=["neglam", "o1"], w=[("ob", par)])
            T.op("dve", lambda: V.tensor_tensor(out=obnf, in0=obf, in1=obf, op=ALU.mult), r=[("ob", par)], w=[("obn", par)])

            def part2():
                sbk = next_gbank()
                T.op("pe", lambda: PE.matmul(ps[sbk][:], lhsT=oneb[:], rhs=obnf, start=True, stop=True), r=[("obn", par), "oneb"], w=[bk(sbk)])
                T.op("act", lambda: A.activation(out=rr[:], in_=ps[sbk][:], func=AF.Ln, scale=1.0 / 128, bias=EPS), w=[bk(sbk), ("rB2", par)])
                T.op("act", lambda: A.activation(out=rr[:], in_=rr[:], func=AF.Exp, scale=-0.5), w=[("rB2", par)])
                gcol = pp[:, 4:5] if isdiff else pp[:, 5:6]
                T.op("dve", lambda: V.scalar_tensor_tensor(out=dstT[:, H, I * 512:(I + 1) * 512], in0=obf, scalar=gcol, in1=rr[:],
                                                           op0=ALU.mult, op1=ALU.mult),
                     r=[("ob", par), ("rB2", par), "pp4", "pp5"], w=[("mixT", H, I)])
            deferred.append([6, part2])

        return len(steps), emit_S, emit_AV

    deferred = []

    order = [4, 5, 6, 7, 0, 1, 2, 3]
    pc0 = proj_chunks(order[0], pbanks=(5, 3, 4))
    qa = [pc0[1 + 2 * k] for k in range(8)]
    qb = [pc0[2 + 2 * k] for k in range(8)]
    qa[0]()
    for k in range(1, 8):
        qa[k]()
        qb[k - 1]()
    qb[7]()
    for ch in pc0[17:]:
        ch()
    heads = {}
    gsteps = []
    pend_of = {}
    for oi, H in enumerate(order):
        nst_h, eS, eAV = attention(H, None, mixT)
        heads[H] = (eS, eAV)
        gsteps += [(H, n) for n in range(nst_h)]
        pend = proj_chunks(order[oi + 1]) if oi < 7 else []
        if oi < 4:
            if oi < 2:
                extra = bt_chunks[8 * oi:8 * oi + 8] + ([bt_finish] if oi == 1 else [])
            else:
                extra = mem_chunks[2 * (oi - 2):2 * (oi - 2) + 2]
            merged = [pend.pop(0)]
            while pend or extra:
                if pend:
                    merged.append(pend.pop(0))
                if pend:
                    merged.append(pend.pop(0))
                if extra:
                    merged.append(extra.pop(0))
            pend = merged
        pend_of[H] = (pend, max(1, nst_h // (len(pend) + 1)) if pend else nst_h)
    infos = {}
    DEPTH = 3
    curH = None
    for g in range(len(gsteps) + DEPTH):
        if g < len(gsteps):
            H, n = gsteps[g]
            if H != curH:
                if curH is not None:
                    while pend_of[curH][0]:
                        pend_of[curH][0].pop(0)()
                curH = H
            infos[g] = heads[H][0](n)
        if g >= DEPTH:
            H2, n2 = gsteps[g - DEPTH]
            heads[H2][1](n2, infos.pop(g - DEPTH))
        for d in list(deferred):
            d[0] -= 1
            if d[0] <= 0:
                deferred.remove(d)
                d[1]()
        if g < len(gsteps):
            pend, every = pend_of[gsteps[g][0]]
            if pend and gsteps[g][1] % every == every - 1:
                pend.pop(0)()
    for d in deferred:
        d[1]()
    T.barrier()
    hs.close()
    xres = sb("xres", [128, NT, D])
    for t in range(NT):
        T.dma("sp", xres[:, t, :], x_d[t * 128:(t + 1) * 128, :], w=[("xres", t)])

    def load_w_full(w_d, slots, key):
        for hf in range(2):
            T.dma("pool", wring[slots[hf]][:].rearrange("p (c n) -> p c n", n=512), wv(w_d)[:, :, hf * 512:(hf + 1) * 512],
                  w=[("wr", slots[hf])])

    def proj_residual(srcT, skeyf, slots, wkey, tiles=range(NT)):
        bi = 0
        for t in tiles:
            for hf in range(2):
                bnk = bi % 3
                bi += 1
                wsl = wring[slots[hf]][:].rearrange("p (c n) -> p c n", n=512)

                def mm(t=t, wsl=wsl, bnk=bnk):
                    for c in range(8):
                        i = PE.matmul(ps[bnk][:], lhsT=srcT[:, c, t * 128:(t + 1) * 128], rhs=wsl[:, c, :], start=(c == 0), stop=(c == 7))
                    return i
                T.op("pe", mm, r=[("wr", slots[hf])] + skeyf(t), w=[bk(bnk)])
                T.op("dve", lambda t=t, hf=hf, bnk=bnk: V.tensor_tensor(out=xres[:, t, hf * 512:(hf + 1) * 512], in0=ps[bnk][:],
                                                                       in1=xres[:, t, hf * 512:(hf + 1) * 512], op=ALU.add),
                     w=[bk(bnk), ("xres", t)])

    load_w_full(w_out_d, (0, 1), "wout")
    wout_keys = lambda t: [("mixT", H, t // 4) for H in range(8)]
    proj_residual(mixT, wout_keys, (0, 1), "wout", tiles=range(0, 8))

    hT2 = sb("hT2", [128, 8, S], BF16)
    PHB = {}

    def pre_b(t):
        def f():
            if t < 4:
                proj_residual(mixT, wout_keys, (0, 1), "wout", tiles=[8 + 2 * t, 9 + 2 * t])
            q0 = PHB.get("q0")
            if t == 4:
                q0[0]()
            if q0 is not None:
                tgq, r = divmod(t - 5, 4)
                if t >= 5 and r == 0 and tgq < 3:
                    q0[1 + 2 * tgq]()
                if t >= 7 and (t - 7) % 4 == 0 and (t - 7) // 4 < 3:
                    q0[2 + 2 * ((t - 7) // 4)]()
        return f
    PHB["pre_b"] = pre_b

    def hT2_keys(tg):
        return [("hT2", tg, c) for c in range(8)]

    crossT = mixT
    gb_list[:] = [0, 1, 2]
    rBc = xn_bufs[0][:].bitcast(F32)

    def qproj_chunks(h, ob=(6, 7)):
        slot = h % 2
        wsl = wring[slot][:, 0:2048].rearrange("p (c n) -> p c n", n=256)
        qc = qk[slot]
        chunks = []

        def load():
            T.dma("pool", wsl, wv(w_cq_d)[:, :, h * 256:(h + 1) * 256], w=[("wr", slot)])
        chunks.append(load)
        for tg in range(4):
            holder = []

            def cha(tg=tg, holder=holder):
                def mmq():
                    for b in range(2):
                        for c in range(8):
                            i = PE.matmul(ps[ob[b]][:], lhsT=wsl[:, c, b * 128:(b + 1) * 128], rhs=hT2[:, c, tg * 512:(tg + 1) * 512],
                                          start=(c == 0), stop=(c == 7))
                    return i
                T.op("pe", mmq, r=[("wr", slot)] + hT2_keys(tg), w=[bk(ob[0]), bk(ob[1])])
                emit_qknorm([ob[0], ob[1]], 256, [pp[:, 6:7], pp[:, 7:8]], [qc[:, 0, tg * 512:(tg + 1) * 512], qc[:, 1, tg * 512:(tg + 1) * 512]],
                            ["pp6"], [[("qk", slot, 0, tg)], [("qk", slot, 1, tg)]], oneb[:], n=512, defer=holder)
            chunks.append(cha)
            chunks.append(lambda holder=holder: holder.pop(0)())
        return chunks

    def cross_attention(h, pending):
        slot = h % 2
        qc = qk[slot]

        def emit_S(tg):
            ets = []
            for mb in range(2):
                sbank = next_gbank()
                etb = st_cnt[0] % 4
                st_cnt[0] += 1
                ets.append(etb)

                def mms(mb=mb, sbank=sbank):
                    for b in range(2):
                        i = PE.matmul(ps[sbank][:], lhsT=kTc[:, h, b, mb * 128:(mb + 1) * 128], rhs=qc[:, b, tg * 512:(tg + 1) * 512],
                                      start=(b == 0), stop=(b == 1))
                    return i
                T.op("pe", mms, r=[("kTc", h), ("qk", slot, 0, tg), ("qk", slot, 1, tg)], w=[bk(sbank)])
                T.op("act", lambda sbank=sbank, etb=etb: A.activation(out=ET[etb][:], in_=ps[sbank][:], func=AF.Exp), w=[bk(sbank), ("ET", etb)])
            return ets

        def emit_AV(tg, ets):
            def mma():
                for b in range(2):
                    for mb in range(2):
                        PE.matmul(ps[3 + b][:], lhsT=vc[:, mb, h, b * 128:(b + 1) * 128], rhs=ET[ets[mb]][:], start=(mb == 0), stop=(mb == 1))
                for mb in range(2):
                    i = PE.matmul(ps[5][:], lhsT=oneb[:], rhs=ET[ets[mb]][:], start=(mb == 0), stop=(mb == 1))
                return i
            T.op("pe", mma, r=[("ET", ets[0]), ("ET", ets[1]), ("vc", 0, h // 2), ("vc", 1, h // 2), "oneb"], w=[bk(3), bk(4), bk(5)])
            T.op("act", lambda: A.activation(out=rBc, in_=ps[5][:], func=AF.Ln), w=[bk(5), ("xn", 0)])
            T.op("act", lambda: A.activation(out=rBc, in_=rBc, func=AF.Exp, scale=-1.0), w=[("xn", 0)])
            for b in range(2):
                T.op("dve", lambda b=b: V.tensor_tensor(out=crossT[:, 2 * h + b, tg * 512:(tg + 1) * 512], in0=ps[3 + b][:], in1=rBc, op=ALU.mult),
                     r=[("xn", 0)], w=[bk(3 + b), ("mixT", 2 * h + b, tg)])

        return emit_S, emit_AV

    q0 = qproj_chunks(0, ob=(3, 4))
    PHB["q0"] = q0
    norm_all([(t, xres[:, t, :], [("xres", t)], PHB["pre_b"](t)) for t in range(NT)], norm_cross_g_d, hT2, "hT2")
    for ch in q0[7:]:
        ch()
    wco_keys = lambda t: [("mixT", cb, t // 4) for cb in range(8)]
    xh = {h: cross_attention(h, None) for h in range(4)}
    xpend = {h: (qproj_chunks(h + 1) if h < 3 else []) for h in range(4)}
    xsteps = [(h, tg) for h in range(4) for tg in range(4)]
    xinfos = {}
    for n in range(len(xsteps) + 1):
        if n < len(xsteps):
            h, tg = xsteps[n]
            if tg == 0 and h > 0:
                while xpend[h - 1]:
                    xpend[h - 1].pop(0)()
            if tg == 0 and h == 3:
                load_w_full(w_co_d, (0, 1), "wco")
            xinfos[n] = xh[h][0](tg)
            for _ in range(3):
                if xpend[h]:
                    xpend[h].pop(0)()
        if n >= 1:
            h2, tg2 = xsteps[n - 1]
            xh[h2][1](tg2, xinfos.pop(n - 1))
            if h2 == 3 and tg2 >= 1:
                proj_residual(crossT, wco_keys, (0, 1), "wco", tiles=range(4 * (tg2 - 1), 4 * (tg2 - 1) + 4))
    proj_residual(crossT, wco_keys, (0, 1), "wco", tiles=range(12, 16))

    T.barrier()
    norm_all([(t, xres[:, t, :], [("xres", t)], None) for t in range(NT)], norm_ffn_g_d, hT2, "hT2")
    wrt = wring[2]
    T.dma("pool", wrt[:, 0:256].rearrange("p (c n) -> p c n", n=32)[:, :, 0:4], wv(w_gr_d), w=[("wr", 2)], allow_slow_non_contiguous=True)
    T.dma("pool", wrt[:, 0:256].rearrange("p (c n) -> p c n", n=32)[:, :, 4:20], wv(w_er_d), w=[("wr", 2)], allow_slow_non_contiguous=True)

    def mmr():
        for t in range(NT):
            for c in range(8):
                i = PE.matmul(ps[0][:, 32 * t:32 * t + 20], lhsT=hT2[:, c, t * 128:(t + 1) * 128], rhs=wrt[:, 32 * c:32 * c + 20],
                              start=(c == 0), stop=(c == 7))
        return i
    T.op("pe", mmr, r=[("wr", 2)] + [k for tg in range(4) for k in hT2_keys(tg)], w=[bk(0)])
    T.op("dve", lambda: V.tensor_tensor(out=rl[:], in0=ps[0][:].rearrange("p (t n) -> p t n", n=32)[:, :, 0:20],
                                        in1=b20[:].unsqueeze(1).broadcast_to([128, NT, 20]), op=ALU.add),
         r=["b20a", "b20b"], w=[bk(0), "rl"])
    RS = rsb[0][:].rearrange("p (t n) -> p t n", n=32)
    z = rl

    def bc(ap2):
        return ap2.unsqueeze(2).broadcast_to([128, NT, 4])

    def dv(fn):
        T.op("dve", fn, r=["rl"], w=["rtr"])

    def ac(fn):
        T.op("act", fn, r=["rl"], w=["rtr"])
    gmax, ngm, sumg, pg = RS[:, :, 0], RS[:, :, 1], RS[:, :, 2], RS[:, :, 3]
    mg, eg, sel, m1, sel2, m2, gin, tmp4 = (RS[:, :, 4 + 4 * i:8 + 4 * i] for i in range(7)) if False else tuple(RS[:, :, 4 + 4 * i:8 + 4 * i] for i in range(7)) + (None,)
    v1, v2, dd, e2 = rt[:, 0:16], rt[:, 16:32], rt[:, 32:48], rt[:, 48:64]
    dv(lambda: V.tensor_reduce(out=gmax, in_=z[:, :, 0:4], axis=AX.X, op=ALU.max))
    dv(lambda: V.tensor_tensor(out=mg, in0=z[:, :, 0:4], in1=bc(gmax), op=ALU.is_equal))
    dv(lambda: V.tensor_tensor(out=eg, in0=z[:, :, 0:4], in1=bc(gmax), op=ALU.subtract))
    ac(lambda: A.activation(out=eg, in_=eg, func=AF.Exp))
    dv(lambda: V.tensor_reduce(out=sumg, in_=eg, axis=AX.X, op=ALU.add))
    dv(lambda: V.reciprocal(out=pg, in_=sumg))
    dv(lambda: V.tensor_tensor(out=sel, in0=z[:, :, 4:8], in1=bc(mg[:, :, 0]), op=ALU.mult))
    for g in range(1, 4):
        dv(lambda g=g: V.tensor_tensor(out=sel2, in0=z[:, :, 4 + 4 * g:8 + 4 * g], in1=bc(mg[:, :, g]), op=ALU.mult))
        dv(lambda: V.tensor_tensor(out=sel, in0=sel, in1=sel2, op=ALU.add))
    dv(lambda: V.tensor_reduce(out=v1, in_=sel, axis=AX.X, op=ALU.max))
    dv(lambda: V.tensor_tensor(out=m1, in0=sel, in1=bc(v1), op=ALU.is_equal))
    dv(lambda: V.scalar_tensor_tensor(out=sel2, in0=m1, scalar=-1e30, in1=sel, op0=ALU.mult, op1=ALU.add))
    dv(lambda: V.tensor_reduce(out=v2, in_=sel2, axis=AX.X, op=ALU.max))
    dv(lambda: V.tensor_tensor(out=m2, in0=sel2, in1=bc(v2), op=ALU.is_equal))
    dv(lambda: V.tensor_tensor(out=dd, in0=v2, in1=v1, op=ALU.subtract))
    ac(lambda: A.activation(out=e2, in_=dd, func=AF.Exp))
    dv(lambda: V.tensor_scalar(out=dd, in0=e2, scalar1=1.0, scalar2=None, op0=ALU.add))
    dv(lambda: V.reciprocal(out=dd, in_=dd))
    dv(lambda: V.tensor_tensor(out=v1, in0=dd, in1=pg, op=ALU.mult))
    dv(lambda: V.tensor_tensor(out=v2, in0=v1, in1=e2, op=ALU.mult))
    dv(lambda: V.tensor_tensor(out=gin, in0=m1, in1=bc(v1), op=ALU.mult))
    dv(lambda: V.tensor_tensor(out=sel2, in0=m2, in1=bc(v2), op=ALU.mult))
    dv(lambda: V.tensor_tensor(out=gin, in0=gin, in1=sel2, op=ALU.add))
    for g in range(4):
        T.op("dve", lambda g=g: V.tensor_tensor(out=gates[:, :, 4 * g:4 * g + 4], in0=gin, in1=bc(mg[:, :, g]), op=ALU.mult),
             r=["rtr"], w=[("gates", t) for t in range(NT)])

    mflat = mixT[:].rearrange("p a b -> p (a b)")
    wslots = [wring[0][:], wring[1][:], wring[2][:], mflat[:, 0:4096], mflat[:, 4096:8192], mflat[:, 8192:12288]]
    aTb = [mflat[:, 12288:14336].rearrange("p (f n) -> p f n", n=512), mflat[:, 14336:16384].rearrange("p (f n) -> p f n", n=512)]
    sil = [sqb[0], sqb[1]]

    def wslot(e, k):
        return wslots[(e % 2) * 3 + k]

    def wxk(i):
        return ("wr", i) if i < 3 else ("wx", i)

    def load_expert(e):
        s3 = (e % 2) * 3
        T.dma("pool", wslot(e, 0).rearrange("p (c n) -> p c n", n=512), wv(w_eg_d[e]), w=[wxk(s3)])
        T.dma("pool", wslot(e, 1).rearrange("p (c n) -> p c n", n=512), wv(w_eu_d[e]), w=[wxk(s3 + 1)])
        T.dma("pool", wslot(e, 2).rearrange("p (c n) -> p c n", n=1024), wv(w_ed_d[e]), w=[wxk(s3 + 2)])

    gu_cnt = [0]

    def emit_GU(e, tg):
        s3 = (e % 2) * 3
        wg = wslot(e, 0).rearrange("p (c n) -> p c n", n=512)
        wu = wslot(e, 1).rearrange("p (c n) -> p c n", n=512)
        ab = (e * 4 + tg) % 2
        for fb in range(4):
            k = gu_cnt[0] % 2
            gu_cnt[0] += 1
            gb, ub = k, 2 + k

            def mmg(fb=fb, gb=gb):
                for c in range(8):
                    i = PE.matmul(ps[gb][:], lhsT=wg[:, c, fb * 128:(fb + 1) * 128], rhs=hT2[:, c, tg * 512:(tg + 1) * 512], start=(c == 0), stop=(c == 7))
                return i

            def mmu(fb=fb, ub=ub):
                for c in range(8):
                    i = PE.matmul(ps[ub][:], lhsT=wu[:, c, fb * 128:(fb + 1) * 128], rhs=hT2[:, c, tg * 512:(tg + 1) * 512], start=(c == 0), stop=(c == 7))
                return i
            T.op("pe", mmg, r=[wxk(s3)] + hT2_keys(tg), w=[bk(gb)])
            T.op("pe", mmu, r=[wxk(s3 + 1)] + hT2_keys(tg), w=[bk(ub)])
            T.op("act", lambda gb=gb, k=k: A.activation(out=sil[k][:], in_=ps[gb][:], func=AF.Silu), w=[bk(gb), ("sil", k)])
            T.op("dve", lambda fb=fb, ub=ub, k=k: V.tensor_tensor(out=aTb[ab][:, fb, :], in0=sil[k][:], in1=ps[ub][:], op=ALU.mult),
                 r=[("sil", k)], w=[bk(ub), ("aT", ab, fb)])

    d_cnt = [0]

    def emit_D(e, tg):
        s3 = (e % 2) * 3
        wd = wslot(e, 2).rearrange("p (c n) -> p c n", n=1024)
        ab = (e * 4 + tg) % 2
        for tt in range(4):
            t = tg * 4 + tt
            for hf in range(2):
                bnk = 4 + d_cnt[0] % 3
                d_cnt[0] += 1

                def mmd(tt=tt, hf=hf, bnk=bnk):
                    for fb in range(4):
                        i = PE.matmul(ps[bnk][:], lhsT=aTb[ab][:, fb, tt * 128:(tt + 1) * 128], rhs=wd[:, fb, hf * 512:(hf + 1) * 512],
                                      start=(fb == 0), stop=(fb == 3))
                    return i
                T.op("pe", mmd, r=[wxk(s3 + 2)] + [("aT", ab, fb) for fb in range(4)], w=[bk(bnk)])
                T.op("dve", lambda t=t, hf=hf, bnk=bnk: V.scalar_tensor_tensor(out=xres[:, t, hf * 512:(hf + 1) * 512], in0=ps[bnk][:],
                                                                              scalar=gates[:, t, e:e + 1], in1=xres[:, t, hf * 512:(hf + 1) * 512],
                                                                              op0=ALU.mult, op1=ALU.add),
                     r=[("gates", t)], w=[bk(bnk), ("xres", t)])
            if e == 15:
                T.dma("sp", out_d[t * 128:(t + 1) * 128, :], xres[:, t, :], r=[("xres", t)], w=[("out", t)])

    load_expert(0)
    stepsC = [(e, tg) for e in range(16) for tg in range(4)]
    for n in range(len(stepsC) + 1):
        if n < len(stepsC):
            e, tg = stepsC[n]
            if tg == 0 and e + 1 < 16:
                pass
            emit_GU(e, tg)
            if tg == 1 and e + 1 < 16:
                load_expert(e + 1)
        if n >= 1:
            emit_D(*stepsC[n - 1])

    T.finish("sp")
    es.close()
    T.close()
    return nc, T


_CACHE = {}


def kernel(**inputs):
    if "nc" not in _CACHE:
        _CACHE["nc"] = build_program()[0]
    nc = _CACHE["nc"]
    cst = _make_consts()
    f32 = lambda a: np.ascontiguousarray(np.asarray(a, dtype=np.float32))
    shared = {"cst": cst}
    for k, v in inputs.items():
        if k in ("x", "mem"):
            continue
        a = f32(v)
        if k == "rel_bias":
            shared[k] = a
        else:
            shared[k] = np.ascontiguousarray(a[0])
    x = f32(inputs["x"])
    mem = f32(inputs["mem"])
    in_maps = []
    for b in range(8):
        m = dict(shared)
        m["x"] = np.ascontiguousarray(x[b])
        m["mem"] = np.ascontiguousarray(mem[b])
        in_maps.append(m)
    res = run_bass_kernel_spmd(nc, in_maps, core_ids=list(range(8)))
    out = np.stack([np.asarray(r["out"], dtype=np.float32) for r in res.results], axis=0)
    return out
```

```python
import math
import numpy as np
import concourse.bass as bass
import concourse.mybir as mybir
from concourse.bass_utils import run_bass_kernel_spmd

F32 = mybir.dt.float32
BF16 = mybir.dt.bfloat16
AF = mybir.ActivationFunctionType
ALU = mybir.AluOpType
AX = mybir.AxisListType

S = 2048
D = 1024
NT = 16
NEG = -30000.0
EPS = 1e-6
ENGS = ["pe", "act", "dve", "pool", "sp"]


class Tracker:
    def __init__(self, nc, n_lanes=9, n_single=88):
        self.nc = nc
        self.engs = {"pe": nc.tensor, "act": nc.scalar, "dve": nc.vector,
                     "pool": nc.gpsimd, "sp": nc.sync}
        self.NL = n_lanes + n_single
        self.n_rr = n_lanes
        self.next_single = 4 + n_lanes
        self.eidx = {e: i for i, e in enumerate(ENGS)}
        self.sems = []
        self._ctx = []
        for i in range(4 + self.NL):
            cm = nc.semaphore(f"s{i}")
            self.sems.append(cm.__enter__())
            self._ctx.append(cm)
        self.NV = 4 + self.NL
        self.cnt = np.zeros(self.NV, dtype=np.int64)
        self.cur = {e: np.zeros(self.NV, dtype=np.int64) for e in ENGS}
        self.last_w = {}
        self.readers = {}
        self.lane_rr = 0
        self.n_waits = 0
        self.n_ops = 0

    def close(self):
        for cm in reversed(self._ctx):
            cm.__exit__(None, None, None)

    def _gather_deps(self, r, w):
        deps = []
        for k in r:
            lw = self.last_w.get(k)
            if lw is not None:
                deps.append(lw)
        for k in w:
            lw = self.last_w.get(k)
            if lw is not None:
                deps.append(lw)
            rd = self.readers.get(k)
            if rd:
                deps.extend(rd.values())
        return deps

    def _emit_waits(self, eng, deps, own_slot):
        cur = self.cur[eng]
        e = self.engs[eng]
        need = {}
        for (slot, c, vc) in deps:
            if slot == own_slot and slot < 4:
                if slot == 0:
                    continue
            if cur[slot] >= c:
                continue
            if need.get(slot, (0, None))[0] < c:
                need[slot] = (c, vc)
        items = sorted(need.items(), key=lambda kv: -kv[1][0])
        for slot, (c, vc) in items:
            if cur[slot] >= c:
                continue
            val = int(c) * (16 if slot >= 4 else 1)
            e.wait_ge(self.sems[slot], val)
            self.n_waits += 1
            np.maximum(cur, vc, out=cur)
            if cur[slot] < c:
                cur[slot] = c

    def _record(self, rec, r, w):
        for k in r:
            self.readers.setdefault(k, {})[rec[0]] = rec
        for k in w:
            self.last_w[k] = rec
            self.readers[k] = {}

    def op(self, eng, fn, r=(), w=()):
        slot = self.eidx[eng]
        deps = self._gather_deps(r, w)
        self._emit_waits(eng, deps, slot)
        inst = fn()
        self.cnt[slot] += 1
        c = int(self.cnt[slot])
        inst.then_inc(self.sems[slot], 1)
        vc = self.cur[eng].copy()
        vc[slot] = c
        rec = (slot, c, vc)
        self._record(rec, r, w)
        self.n_ops += 1
        return rec

    def dma(self, q, out, in_, r=(), w=(), **kw):
        if q == "pool":
            slot = self.next_single
            self.next_single += 1
            assert slot < self.NV, "out of single-use DMA semaphores"
        else:
            lane = self.lane_rr
            self.lane_rr = (self.lane_rr + 1) % self.n_rr
            slot = 4 + lane
        deps = self._gather_deps(r, w)
        prev = int(self.cnt[slot])
        if prev > 0:
            pvc = np.zeros(self.NV, dtype=np.int64)
            pvc[slot] = prev
            deps.append((slot, prev, pvc))
        self._emit_waits(q, deps, -1)
        inst = self.engs[q].dma_start(out=out, in_=in_, **kw)
        self.cnt[slot] += 1
        c = int(self.cnt[slot])
        inst.then_inc(self.sems[slot], 16)
        vc = self.cur[q].copy()
        vc[slot] = c
        rec = (slot, c, vc)
        self._record(rec, r, w)
        self.n_ops += 1
        return rec

    def barrier(self):
        full = self.cnt.copy()
        for eng in ENGS:
            cur = self.cur[eng]
            e = self.engs[eng]
            own = self.eidx[eng]
            for slot in range(self.NV):
                if slot == own and slot < 4:
                    continue
                if cur[slot] < full[slot]:
                    val = int(full[slot]) * (16 if slot >= 4 else 1)
                    e.wait_ge(self.sems[slot], val)
                    self.n_waits += 1
                    cur[slot] = full[slot]
        self.last_w.clear()
        self.readers.clear()

    def finish(self, eng="sp"):
        cur = self.cur[eng]
        e = self.engs[eng]
        for slot in range(self.NV):
            if cur[slot] < self.cnt[slot]:
                val = int(self.cnt[slot]) * (16 if slot >= 4 else 1)
                e.wait_ge(self.sems[slot], val)
                cur[slot] = self.cnt[slot]


def _t5_bucket_np(rel):
    nb = 16
    max_exact = 8
    ret = (rel > 0).astype(np.int32) * nb
    n = np.abs(rel)
    nf = np.maximum(n, 1).astype(np.float32)
    large = max_exact + (np.log(nf / np.float32(max_exact)) / np.float32(math.log(128 / max_exact))
                         * np.float32(nb - max_exact)).astype(np.int32)
    large = np.minimum(large, nb - 1)
    return ret + np.where(n < max_exact, n, large)


C_ID, C_BLK, C_ONE, C_TRI, C_SEL, C_MD, C_MF, C_G = 0, 128, 256, 384, 512, 640, 768, 896
C_W = 896 + 384


def _make_consts():
    c = np.zeros((128, C_W), dtype=np.float32)
    p = np.arange(128)[:, None]
    f = np.arange(128)[None, :]
    c[:, C_ID:C_ID + 128] = (p == f)
    c[:, C_BLK:C_BLK + 128] = ((p // 64) == (f // 64))
    c[:, C_ONE:C_ONE + 128] = 1.0
    c[:, C_TRI:C_TRI + 128] = (p <= f)
    c[:, C_SEL:C_SEL + 128] = (p == 127)
    c[:, C_MD:C_MD + 128] = np.where((p >= 64) & (f < 64), NEG, 0.0)
    c[:, C_MF:C_MF + 128] = np.where(p <= f, 0.0, NEG)
    u = np.arange(383)
    rel = u - 255
    bk = _t5_bucket_np(rel.astype(np.int32))
    g = np.zeros((32, 384), dtype=np.float32)
    g[bk, u] = 1.0
    c[0:32, C_G:C_G + 384] = g
    return c


def build_program():
    nc = bass.Bass("TRN2", target_bir_lowering=False)

    def din(name, shape):
        return nc.dram_tensor(name, list(shape), F32, kind="ExternalInput").ap()

    x_d = din("x", [S, D])
    mem_d = din("mem", [256, D])
    rel_bias_d = din("rel_bias", [32, 4])
    norm_mix_g_d = din("norm_mix_g", [D])
    w_in_d = din("w_in", [D, 3076])
    b_forget_d = din("b_forget", [4])
    dqg_d = din("diff_q_norm_g", [64])
    dkg_d = din("diff_k_norm_g", [64])
    lq1_d = din("diff_lambda_q1", [64])
    lk1_d = din("diff_lambda_k1", [64])
    lq2_d = din("diff_lambda_q2", [64])
    lk2_d = din("diff_lambda_k2", [64])
    dsub_d = din("diff_subln_g", [128])
    fqg_d = din("fox_q_norm_g", [128])
    fkg_d = din("fox_k_norm_g", [128])
    fog_d = din("fox_out_norm_g", [128])
    w_out_d = din("w_out", [D, D])
    norm_cross_g_d = din("norm_cross_g", [D])
    norm_mem_g_d = din("norm_mem_g", [D])
    w_cq_d = din("w_cq", [D, D])
    w_ckv_d = din("w_ckv", [D, 2 * D])
    cqg_d = din("cross_q_norm_g", [256])
    ckg_d = din("cross_k_norm_g", [256])
    w_co_d = din("w_co", [D, D])
    norm_ffn_g_d = din("norm_ffn_g", [D])
    w_gr_d = din("w_group_router", [D, 4])
    b_gr_d = din("b_group_router", [4])
    w_er_d = din("w_expert_router", [D, 16])
    b_er_d = din("b_expert_router", [16])
    w_eg_d = din("w_exp_gate", [16, D, 512])
    w_eu_d = din("w_exp_up", [16, D, 512])
    w_ed_d = din("w_exp_down", [16, 512, D])
    cst_d = din("cst", [128, C_W])
    out_d = nc.dram_tensor("out", [S, D], F32, kind="ExternalOutput").ap()

    T = Tracker(nc)
    V, A, PE = nc.vector, nc.scalar, nc.tensor
    LAM_INIT = 0.8 - 0.6 * math.exp(0.0)

    def wv(ap):
        return ap.rearrange("(c p) n -> p c n", p=128)

    from contextlib import ExitStack
    es = ExitStack()

    def sb(name, shape, dt=F32):
        return es.enter_context(nc.sbuf_tensor(name, list(shape), dt))

    cst = sb("cst_sb", [128, C_W])
    identb = sb("identb", [128, 128], BF16)
    blkb = sb("blkb", [128, 128], BF16)
    oneb = sb("oneb", [128, 128], BF16)
    gn = sb("gn", [128, 4, 8])
    pp = sb("pp", [128, 16])
    lams = sb("lams", [128, 8])
    b20 = sb("b20", [128, 20])
    sqb = [sb(f"sq{i}", [128, 512], BF16) for i in range(2)]
    rsb = [sb(f"rs{i}", [128, 512]) for i in range(2)]
    ET = [sb(f"ET{i}", [128, 512], BF16) for i in range(6)]
    qk = [sb(f"qk{i}", [128, 2, S], BF16) for i in range(2)]
    obn = sb("obn", [128, 4, 128], BF16)
    sm = sb("sm", [128, 32])
    wring = [sb(f"wr{i}", [128, 4096], BF16) for i in range(3)]
    rl = sb("rl", [128, 16, 20])
    gates = sb("gates", [128, 16, 16])
    rt = sb("rt", [128, 64])
    xn_bufs = [sb(f"xn{i}", [128, D], BF16) for i in range(2)]
    junk = sb("junk", [128, D], BF16)
    kTc = sb("kTc", [128, 4, 2, 256], BF16)
    vc = sb("vc", [128, 2, 4, 256], BF16)
    mixT = sb("mixT", [128, 8, S], BF16)
    hs = ExitStack()

    def sba(name, shape, dt=F32):
        return hs.enter_context(nc.sbuf_tensor(name, list(shape), dt))
    lamw = sba("lamw", [128, 4, 64])
    bfo = sba("bfo", [128, 4])
    rb = sba("rb", [32, 4])
    c15 = sba("c15", [128, 4])
    BT = sba("BT", [128, 4, 5, 128])
    fl = sba("fl", [128, 16, 4])
    cumL = sba("cumL", [128, 16, 4])
    carry = sba("carry", [128, 16, 4])
    cref = sba("cref", [128, 16, 4])
    fbt = sba("fbt", [128, 4, 16, 16])
    tmpn = [sba(f"tmpn{i}", [128, 512]) for i in range(2)]
    vb = [sba(f"vb{i}", [128, 16, 128], BF16) for i in range(2)]
    qz = [sba(f"qz{i}", [128, S], BF16) for i in range(2)]
    o1 = sba("o1", [128, 4, 128])
    ob = sba("ob", [128, 4, 128])
    rB = sba("rB", [128, 512])
    mf2 = sba("mf2", [128, 256])
    obB = sba("obB", [128, 4, 128])
    obnB = sba("obnB", [128, 4, 128], BF16)
    rB2 = [sba(f"rB2_{i}", [128, 512]) for i in range(2)]
    memT = sba("memT", [128, 8, 256], BF16)
    hT = sba("hT", [128, 8, S], BF16)
    xs = [sba(f"xs{i}", [128, D], F32) for i in range(2)]

    ps = [es.enter_context(nc.psum_tensor(f"ps{i}", [128, 512], F32)) for i in range(8)]
    psT = ps[7][:].bitcast(BF16)
    gb_cnt = [0]

    gb_list = [0, 1, 2, 6, 7]

    def next_gbank():
        b = gb_list[gb_cnt[0] % len(gb_list)]
        gb_cnt[0] += 1
        return b

    def bk(i):
        return ("bank", i)

    T.dma("sp", cst[:], cst_d[:, :], w=["cst"])
    T.op("dve", lambda: V.tensor_copy(out=identb[:], in_=cst[:, C_ID:C_ID + 128]), r=["cst"], w=["identb"])
    T.op("dve", lambda: V.tensor_copy(out=blkb[:], in_=cst[:, C_BLK:C_BLK + 128]), r=["cst"], w=["blkb"])
    T.op("dve", lambda: V.tensor_copy(out=oneb[:], in_=cst[:, C_ONE:C_ONE + 128]), r=["cst"], w=["oneb"])
    bt_chunks = []
    for t in range(2):
        for f0 in range(0, 128, 16):
            def mmb(t=t, f0=f0):
                if t == 1 and f0 >= 96:
                    for h in range(4):
                        T.op("dve", lambda h=h: V.tensor_copy(out=BT[:, h, 1, f0:f0 + 16], in_=c15[:, h:h + 1].broadcast_to([128, 16])),
                             r=["c15"], w=[("BT", h)])
                    return
                gbk = next_gbank()

                def mm():
                    for f in range(f0, f0 + 16):
                        u0 = 255 - f - 128 * t
                        i = PE.matmul(ps[gbk][:, 4 * (f - f0):4 * (f - f0) + 4], lhsT=cst[0:32, C_G + u0:C_G + u0 + 128],
                                      rhs=rb[:, :], start=True, stop=True)
                    return i
                T.op("pe", mm, r=["cst", "rb"], w=[bk(gbk)])
                for h in range(4):
                    src = ps[gbk][:, 0:64].rearrange("p (f h) -> p f h", h=4)[:, :, h]
                    T.op("dve", lambda h=h, src=src: V.tensor_copy(out=BT[:, h, t, f0:f0 + 16], in_=src), w=[bk(gbk), ("BT", h)])
            bt_chunks.append(mmb)

    def bt_finish():
        for h in range(4):
            T.op("dve", lambda h=h: V.tensor_tensor(out=BT[:, h, 0, :], in0=BT[:, h, 0, :], in1=cst[:, C_MD:C_MD + 128], op=ALU.add),
                 r=["cst"], w=[("BT", h)])
            T.op("dve", lambda h=h: V.tensor_copy(out=BT[:, h, 2:5, :].rearrange("p t q -> p (t q)"),
                                                  in_=c15[:, h:h + 1].broadcast_to([128, 384])),
                 r=["c15"], w=[("BT", h)])

    def rstd_small(src_ss, n, scale, keyr, dst, keyw):
        T.op("act", lambda: A.activation(out=dst, in_=src_ss, func=AF.Ln, scale=scale, bias=EPS), r=[keyr], w=[keyw])
        T.op("act", lambda: A.activation(out=dst, in_=dst, func=AF.Exp, scale=-0.5), r=[keyw], w=[keyw])

    nrm_cnt = [0]

    gB = qk[1][:, 0, :].bitcast(F32)
    GBK = [("qk", 1, 0, tg) for tg in range(4)]

    def norm_stage1(src_ap, src_keys, n):
        b = n % 2
        sc = sm[:, 16 + 2 * b:16 + 2 * b + 1]
        sr = sm[:, 17 + 2 * b:17 + 2 * b + 1]
        T.op("act", lambda: A.activation(out=junk[:], in_=src_ap, func=AF.Square, accum_out=sc), r=src_keys, w=["junk", ("nss", b)])
        rstd_small(sc, 1, 1.0 / D, ("nss", b), sr, ("nrs", b))
        T.op("dve", lambda: V.scalar_tensor_tensor(out=xn_bufs[b][:], in0=src_ap, scalar=sr, in1=gB, op0=ALU.mult, op1=ALU.mult),
             r=list(src_keys) + [("nrs", b)] + GBK, w=[("xn", b)])

    def norm_stage2(n, dstT, dkey, t):
        b = n % 2
        nb_ = 6 + b
        pT = ps[nb_][:].bitcast(BF16)

        def tr():
            for c in range(8):
                i = PE.transpose(out=pT[:, c * 128:(c + 1) * 128], in_=xn_bufs[b][:, c * 128:(c + 1) * 128], identity=identb[:])
            return i
        T.op("pe", tr, r=[("xn", b), "identb"], w=[bk(nb_)])
        T.op("dve", lambda: V.tensor_copy(out=dstT[:, :, t * 128:(t + 1) * 128], in_=pT.rearrange("p (c n) -> p c n", n=128)),
             w=[bk(nb_)] + [(dkey, t // 4, c) for c in range(8)])

    def norm_all(items, g_d, dstT, dkey):
        T.dma("sp", gB, g_d.partition_broadcast(128), w=GBK)
        prev = None
        for (t, src_ap, src_keys, pre) in items:
            if pre is not None:
                pre()
            n = nrm_cnt[0]
            nrm_cnt[0] += 1
            norm_stage1(src_ap, src_keys, n)
            if prev is not None:
                norm_stage2(prev[0], dstT, dkey, prev[1])
            prev = (n, t)
        norm_stage2(prev[0], dstT, dkey, prev[1])

    qkn_cnt = [0]

    def emit_qknorm(banks, dk, gcols, dsts, rkeys, wkeys, blk_ap, n=512, ssbank=None, defer=None, split=None):
        i0 = qkn_cnt[0]
        qkn_cnt[0] += 1
        sqs = []
        for bi, bnk in enumerate(banks):
            sq = sqb[i0 % 2] if len(banks) == 1 else sqb[bi]
            sqs.append(sq)
            T.op("act", lambda bnk=bnk, sq=sq: A.activation(out=sq[:, 0:n], in_=ps[bnk][:, 0:n], func=AF.Square),
                 w=[bk(bnk), ("sq", id(sq))])

        def part_b():
            _qknorm_b(banks, dk, gcols, dsts, rkeys, wkeys, blk_ap, n, (next_gbank() if ssbank is None else ssbank), sqs, i0, split)
        if defer is not None:
            defer.append(part_b)
        else:
            part_b()

    def _qknorm_b(banks, dk, gcols, dsts, rkeys, wkeys, blk_ap, n, ssbank, sqs, i0, split):
        def mss():
            for bi, sq in enumerate(sqs):
                i = PE.matmul(ps[ssbank][:, 0:n], lhsT=blk_ap, rhs=sq[:, 0:n], start=(bi == 0), stop=(bi == len(sqs) - 1))
            return i
        T.op("pe", mss, r=[("sq", id(sq)) for sq in sqs] + ["blkb", "oneb"], w=[bk(ssbank)])
        rs = rsb[i0 % 2]
        T.op("act", lambda: A.activation(out=rs[:, 0:n], in_=ps[ssbank][:, 0:n], func=AF.Ln, scale=1.0 / dk, bias=EPS),
             w=[bk(ssbank), ("rs", i0 % 2)])
        T.op("act", lambda: A.activation(out=rs[:, 0:n], in_=rs[:, 0:n], func=AF.Exp, scale=-0.5), w=[("rs", i0 % 2)])
        if split is not None:
            bnk = banks[0]
            for (lo, hi, dst, wk) in split:
                T.op("dve", lambda lo=lo, hi=hi, dst=dst: V.scalar_tensor_tensor(out=dst, in0=ps[bnk][lo:hi, 0:n], scalar=gcols[0][lo:hi],
                                                                                  in1=rs[lo:hi, 0:n], op0=ALU.mult, op1=ALU.mult),
                     r=[("rs", i0 % 2)] + list(rkeys), w=[bk(bnk)] + list(wk))
            return
        for bi, bnk in enumerate(banks):
            T.op("dve", lambda bi=bi, bnk=bnk: V.scalar_tensor_tensor(out=dsts[bi], in0=ps[bnk][:, 0:n], scalar=gcols[bi],
                                                                       in1=rs[:, 0:n], op0=ALU.mult, op1=ALU.mult),
                 r=[("rs", i0 % 2)] + list(rkeys), w=[bk(bnk)] + list(wkeys[bi]))

    def pre_a(t):
        def f():
            T.dma("sp", xs[t % 2][:], x_d[t * 128:(t + 1) * 128, :], w=[("xs", t % 2)])
        return f
    norm_all([(t, xs[t % 2][:], [("xs", t % 2)], pre_a(t)) for t in range(NT)], norm_mix_g_d, hT, "hT")

    wq_early = wring[0][:, 0:3072].rearrange("p (c n) -> p c n", n=384)
    for i_, c0_ in enumerate((1536, 2048, 2560)):
        T.dma("pool", wq_early[:, :, i_ * 128:(i_ + 1) * 128], wv(w_in_d)[:, :, c0_:c0_ + 128], w=[("wr", 0)])
    def col(ap):
        return ap.rearrange("(p o) -> p o", o=1)
    T.dma("sp", pp[0:64, 0:1], col(dqg_d), w=["pp0"], allow_slow_non_contiguous=True)
    T.dma("sp", pp[64:128, 0:1], col(dqg_d), w=["pp0"], allow_slow_non_contiguous=True)
    T.dma("sp", pp[0:64, 1:2], col(dkg_d), w=["pp1"], allow_slow_non_contiguous=True)
    T.dma("sp", pp[64:128, 1:2], col(dkg_d), w=["pp1"], allow_slow_non_contiguous=True)
    T.dma("sp", pp[:, 2:3], col(fqg_d), w=["pp2"], allow_slow_non_contiguous=True)
    T.dma("sp", pp[:, 3:4], col(fkg_d), w=["pp3"], allow_slow_non_contiguous=True)
    T.dma("sp", pp[:, 4:5], col(dsub_d), w=["pp4"], allow_slow_non_contiguous=True)
    T.dma("sp", pp[:, 5:6], col(fog_d), w=["pp5"], allow_slow_non_contiguous=True)
    T.dma("sp", pp[:, 6:8], cqg_d.rearrange("(c p) -> p c", p=128), w=["pp6"], allow_slow_non_contiguous=True)
    T.dma("sp", pp[:, 8:10], ckg_d.rearrange("(c p) -> p c", p=128), w=["pp8"], allow_slow_non_contiguous=True)
    T.op("dve", lambda: V.tensor_scalar(out=pp[:, 0:1], in0=pp[:, 0:1], scalar1=64 ** -0.5, scalar2=None, op0=ALU.mult), r=["pp0"], w=["pp0"])
    T.op("dve", lambda: V.tensor_scalar(out=pp[:, 2:3], in0=pp[:, 2:3], scalar1=128 ** -0.5, scalar2=None, op0=ALU.mult), r=["pp2"], w=["pp2"])
    T.op("dve", lambda: V.tensor_scalar(out=pp[:, 4:5], in0=pp[:, 4:5], scalar1=1.0 - LAM_INIT, scalar2=None, op0=ALU.mult), r=["pp4"], w=["pp4"])
    T.op("dve", lambda: V.tensor_scalar(out=pp[:, 6:8], in0=pp[:, 6:8], scalar1=256 ** -0.5, scalar2=None, op0=ALU.mult), r=["pp6"], w=["pp6"])
    for i, l_d in enumerate([lq1_d, lk1_d, lq2_d, lk2_d]):
        T.dma("sp", lamw[:, i, :], l_d.partition_broadcast(128), w=[("lamw", i)])
    T.op("dve", lambda: V.tensor_tensor(out=lamw[:, 0, :], in0=lamw[:, 0, :], in1=lamw[:, 1, :], op=ALU.mult), r=[("lamw", 1)], w=[("lamw", 0)])
    T.op("dve", lambda: V.tensor_tensor(out=lamw[:, 2, :], in0=lamw[:, 2, :], in1=lamw[:, 3, :], op=ALU.mult), r=[("lamw", 3)], w=[("lamw", 2)])
    T.op("dve", lambda: V.reduce_sum(out=lams[:, 0:1], in_=lamw[:, 0, :], axis=AX.X), r=[("lamw", 0)], w=["lams0"])
    T.op("dve", lambda: V.reduce_sum(out=lams[:, 1:2], in_=lamw[:, 2, :], axis=AX.X), r=[("lamw", 2)], w=["lams1"])
    T.op("act", lambda: A.activation(out=lams[:, 2:4], in_=lams[:, 0:2], func=AF.Exp), r=["lams0", "lams1"], w=["lams2"])
    T.op("dve", lambda: V.tensor_tensor(out=lams[:, 4:5], in0=lams[:, 2:3], in1=lams[:, 3:4], op=ALU.subtract), r=["lams2"], w=["lams4"])
    T.op("dve", lambda: V.tensor_scalar(out=lams[:, 5:6], in0=lams[:, 4:5], scalar1=-1.0, scalar2=-LAM_INIT, op0=ALU.mult, op1=ALU.add), r=["lams4"], w=["neglam"])
    T.dma("sp", bfo[:], b_forget_d.partition_broadcast(128), w=["bfo"])
    T.dma("sp", b20[:, 0:4], b_gr_d.partition_broadcast(128), w=["b20a"])
    T.dma("sp", b20[:, 4:20], b_er_d.partition_broadcast(128), w=["b20b"])
    T.dma("sp", rb[:], rel_bias_d[:, :], w=["rb"])
    T.dma("sp", c15[:], rel_bias_d[15:16, :].partition_broadcast(128).rearrange("p o h -> p (o h)"), w=["c15"])


    def hT_keys(tg):
        return [("hT", tg, c) for c in range(8)]

    wf = wring[2]
    T.dma("pool", wf[:, 0:32].rearrange("p (c n) -> p c n", n=4), wv(w_in_d)[:, :, 3072:3076], w=[("wr", 2)],
          allow_slow_non_contiguous=True)

    def mmf():
        for t in range(NT):
            for c in range(8):
                i = PE.matmul(ps[5][:, 4 * t:4 * t + 4], lhsT=hT[:, c, t * 128:(t + 1) * 128], rhs=wf[:, 4 * c:4 * c + 4],
                              start=(c == 0), stop=(c == 7))
        return i
    T.op("pe", mmf, r=[("wr", 2)] + [k for tg in range(4) for k in hT_keys(tg)], w=[bk(5)])
    for h in range(4):
        src = ps[5][:, 0:64].rearrange("p (t h) -> p t h", h=4)[:, :, h]
        T.op("dve", lambda h=h, src=src: V.tensor_scalar(out=fl[:, :, h], in0=src, scalar1=bfo[:, h:h + 1], scalar2=None, op0=ALU.add),
             r=["bfo"], w=[bk(5), "fl"])
    flf = fl[:].rearrange("p t h -> p (t h)")
    T.op("act", lambda: A.activation(out=flf, in_=flf, func=AF.Exp, scale=-1.0), w=["fl"])
    T.op("act", lambda: A.activation(out=flf, in_=flf, func=AF.Ln, bias=1.0), w=["fl"])
    T.op("pe", lambda: PE.matmul(ps[5][:, 0:64], lhsT=cst[:, C_TRI:C_TRI + 128], rhs=flf, start=True, stop=True),
         r=["fl", "cst"], w=[bk(5)])
    T.op("pe", lambda: PE.matmul(ps[6][:, 0:64], lhsT=cst[:, C_ONE:C_ONE + 128], rhs=flf, start=True, stop=True),
         r=["fl", "cst"], w=[bk(6)])
    T.op("dve", lambda: V.memset(carry[:, 0, :], 0.0), w=["carry"])
    totv = ps[6][:, 0:64].rearrange("p (t h) -> p t h", h=4)
    for j in range(1, NT):
        T.op("dve", lambda j=j: V.tensor_tensor(out=carry[:, j, :], in0=carry[:, j - 1, :], in1=totv[:, j - 1, :], op=ALU.add),
             w=["carry", bk(6)])
    T.op("dve", lambda: V.tensor_tensor(out=cumL[:].rearrange("p t h -> p (t h)"), in0=ps[5][:, 0:64],
                                        in1=carry[:].rearrange("p t h -> p (t h)"), op=ALU.add), r=["carry"], w=[bk(5), "cumL"])
    T.op("pe", lambda: PE.matmul(ps[5][:, 0:64], lhsT=cst[:, C_SEL:C_SEL + 128], rhs=cumL[:].rearrange("p t h -> p (t h)"),
                                 start=True, stop=True), r=["cumL", "cst"], w=[bk(5)])
    T.op("dve", lambda: V.tensor_scalar(out=cref[:].rearrange("p t h -> p (t h)"), in0=ps[5][:, 0:64], scalar1=-1.0, scalar2=None,
                                        op0=ALU.mult), w=[bk(5), "cref"])
    for h in range(4):
        T.op("dve", lambda h=h: V.tensor_tensor(out=fbt[:, h, :, :], in0=cumL[:, :, h].unsqueeze(2).broadcast_to([128, NT, NT]),
                                                in1=cref[:, :, h].unsqueeze(1).broadcast_to([128, NT, NT]), op=ALU.add),
             r=["cref", "cumL"], w=[("fbt", h)])

    def pre_m(mt):
        return lambda: T.dma("sp", xs[mt][:], mem_d[mt * 128:(mt + 1) * 128, :], w=[("xs", mt)])
    norm_all([(mt, xs[mt][:], [("xs", mt)], pre_m(mt)) for mt in range(2)], norm_mem_g_d, memT, "memT")
    memkeys = [("memT", 0, c) for c in range(8)]
    def mem_piece(piece):
        T.dma("pool", wring[2][:].rearrange("p (c n) -> p c n", n=512), wv(w_ckv_d)[:, :, piece * 512:(piece + 1) * 512], w=[("wr", 2)])
        wsl = wring[2][:].rearrange("p (c n) -> p c n", n=512)
        if piece < 2:
            for hh in range(2):
                h = piece * 2 + hh

                def mmk(hh=hh):
                    for b in range(2):
                        for c in range(8):
                            i = PE.matmul(ps[b][:, 0:256], lhsT=wsl[:, c, hh * 256 + b * 128:hh * 256 + (b + 1) * 128], rhs=memT[:, c, :],
                                          start=(c == 0), stop=(c == 7))
                    return i
                T.op("pe", mmk, r=[("wr", 2)] + memkeys, w=[bk(0), bk(1)])
                emit_qknorm([0, 1], 256, [pp[:, 8:9], pp[:, 9:10]], [kTc[:, h, 0, :], kTc[:, h, 1, :]], ["pp8"],
                            [[("kTc", h)], [("kTc", h)]], oneb[:], n=256, ssbank=2)
        else:
            half = piece - 2
            for mt in range(2):
                def mmv(mt=mt):
                    for c in range(8):
                        i = PE.matmul(ps[mt][:], lhsT=memT[:, c, mt * 128:(mt + 1) * 128], rhs=wsl[:, c, :], start=(c == 0), stop=(c == 7))
                    return i
                T.op("pe", mmv, r=[("wr", 2)] + memkeys, w=[bk(mt)])
                T.op("dve", lambda mt=mt, half=half: V.tensor_copy(out=vc[:, mt, 2 * half:2 * half + 2, :],
                                                                  in_=ps[mt][:].rearrange("p (a b) -> p a b", b=256)),
                     w=[bk(mt), ("vc", mt, half)])
    mem_chunks = [(lambda p=p: mem_piece(p)) for p in range(4)]
    T.op("dve", lambda: V.tensor_copy(out=mf2[:, 0:128], in_=cst[:, C_MF:C_MF + 128]), r=["cst"], w=["mf2"])
    T.op("pool", lambda: nc.gpsimd.memset(mf2[:, 128:256], 0.0), w=["mf2"])

    for i in range(2):
        T.op("pool", lambda i=i: nc.gpsimd.memset(qk[i][64:128, 0, :], 0.0), w=[("qk", i, 0, tg) for tg in range(4)])
        T.op("pool", lambda i=i: nc.gpsimd.memset(qz[i][0:64, :], 0.0), w=[("qz", i, tg) for tg in range(4)])


    def head_cols(H):
        if H < 4:
            return (H * 128, 512 + H * 128, 1024 + H * 128)
        h = H - 4
        return (1536 + h * 128, 2048 + h * 128, 2560 + h * 128)

    def proj_chunks(H, pbanks=(5,)):
        slot = H % 2
        wsl = wring[slot]
        cq, ck, cv = head_cols(H)
        wq = wsl[:, 0:3072].rearrange("p (c n) -> p c n", n=384)
        chunks = []

        isdiff = H < 4

        def load():
            for i, c0 in enumerate((cq, ck, cv)):
                T.dma("pool", wq[:, :, i * 128:(i + 1) * 128], wv(w_in_d)[:, :, c0:c0 + 128], w=[("wr", slot)])
            if isdiff:
                T.op("pool", lambda: nc.gpsimd.memset(qk[slot][64:128, 0, :], 0.0), w=[("qk", slot, 0, tg) for tg in range(4)])
        chunks.append(load)
        dk = 64 if isdiff else 128
        blk_ap = blkb[:] if isdiff else oneb[:]
        for which in range(2):
            gcol = pp[:, (0 if isdiff else 2) + which:(0 if isdiff else 2) + which + 1]
            gkey = f"pp{(0 if isdiff else 2) + which}"
            for tg in range(4):
                pb = pbanks[(which * 4 + tg) % len(pbanks)]

                def ch(which=which, tg=tg, gcol=gcol, gkey=gkey, holder=None, pb=pb):
                    def mm():
                        for c in range(8):
                            i = PE.matmul(ps[pb][:], lhsT=wq[:, c, which * 128:(which + 1) * 128], rhs=hT[:, c, tg * 512:(tg + 1) * 512],
                                          start=(c == 0), stop=(c == 7))
                        return i
                    T.op("pe", mm, r=[("wr", slot)] + hT_keys(tg), w=[bk(pb)])
                    cs_ = slice(tg * 512, (tg + 1) * 512)
                    sp = None
                    if isdiff and which == 0:
                        sp = [(0, 64, qk[slot][0:64, 0, cs_], [("qk", slot, 0, tg)]),
                              (64, 128, qz[slot][64:128, cs_], [("qz", slot, tg)])]
                    emit_qknorm([pb], dk, [gcol], [qk[slot][:, which, cs_]], [gkey],
                                [[("qk", slot, which, tg)]], blk_ap, defer=holder, split=sp)
                holder = []
                ch.__defaults__ = ch.__defaults__
                chunks.append((lambda ch=ch, holder=holder: ch(holder=holder)))
                chunks.append((lambda holder=holder: holder.pop(0)()))
        for tg in range(4):
            def chv(tg=tg):
                def mm():
                    for tt in range(4):
                        t = tg * 4 + tt
                        for c in range(8):
                            i = PE.matmul(ps[5][:, tt * 128:(tt + 1) * 128], lhsT=hT[:, c, t * 128:(t + 1) * 128],
                                          rhs=wq[:, c, 256:384], start=(c == 0), stop=(c == 7))
                    return i
                T.op("pe", mm, r=[("wr", slot)] + hT_keys(tg), w=[bk(5)])
                T.op("dve", lambda: V.tensor_copy(out=vb[slot][:, tg * 4:(tg + 1) * 4, :],
                                                  in_=ps[5][:].rearrange("p (a b) -> p a b", b=128)),
                     w=[bk(5), ("vb", slot, tg)])
            chunks.append(chv)
        return chunks

    st_cnt = [0]
    fin_cnt = [0]

    def attention(H, pending, dstT):
        slot = H % 2
        isdiff = H < 4
        h = H if isdiff else H - 4
        qT = qk[slot][:, 0, :]
        kT = qk[slot][:, 1, :]
        vv = vb[slot]
        comps = [0, 1] if isdiff else [0]
        steps = [(I, c, j) for I in range(4) for c in comps for j in range(4 * I + 4)]

        def acc_ap(il):
            return ps[3 + il // 2][:, (il % 2) * 256:(il % 2) * 256 + 129]

        def emit_S(n):
            I, c, j = steps[n]
            i0 = max(4 * I, j)
            ncol = (4 * I + 4 - i0) * 128
            q0 = i0 * 128
            sbank = next_gbank()
            etb = st_cnt[0] % 6
            st_cnt[0] += 1
            et = ET[etb]
            if isdiff and c == 1:
                rk = [("qz", slot, I), ("qk", slot, 1, j // 4)]
                qsrc = qz[slot]
            else:
                rk = [("qk", slot, 0, I), ("qk", slot, 1, j // 4)]
                qsrc = qT
            T.op("pe", lambda: PE.matmul(ps[sbank][:, 0:ncol], lhsT=kT[:, j * 128:(j + 1) * 128], rhs=qsrc[:, q0:q0 + ncol],
                                         start=True, stop=True), r=rk, w=[bk(sbank)])
            nb = ncol // 128
            late = []
            if isdiff:
                t_lo = i0 - j
                n_near = 0 if t_lo >= 2 else min(nb, 2 - t_lo)
                if n_near > 0:
                    tb = tmpn[n % 2]
                    tkey = ("tmpn", n % 2)
                    bsrc = BT[:, h, t_lo:t_lo + nb, :].rearrange("p t q -> p (t q)")
                    T.op("dve", lambda: V.tensor_tensor(out=tb[:, 0:ncol], in0=ps[sbank][:, 0:ncol], in1=bsrc, op=ALU.add),
                         r=[("BT", h)], w=[bk(sbank), tkey])
                    late.append((lambda: A.activation(out=et[:, 0:ncol], in_=tb[:, 0:ncol], func=AF.Exp), [tkey]))
                else:
                    T.op("act", lambda: A.activation(out=et[:, 0:ncol], in_=ps[sbank][:, 0:ncol], func=AF.Exp, bias=c15[:, h:h + 1]),
                         r=["c15"], w=[bk(sbank), ("ET", etb)])
            else:
                runs = []
                merged_pair = [None]
                for bi in range(nb):
                    i = i0 + bi
                    pr = i // 2
                    if i == j:
                        wd = 256 if (i % 2 == 0 and bi + 1 < nb) else 128
                        if wd == 256:
                            merged_pair[0] = pr
                        cs = slice(bi * 128, bi * 128 + wd)
                        tb = tmpn[n % 2]
                        tkey = ("tmpn", n % 2)
                        T.op("dve", lambda cs=cs, tb=tb, wd=wd: V.tensor_tensor(out=tb[:, 0:wd], in0=ps[sbank][:, cs], in1=mf2[:, 0:wd], op=ALU.add),
                             r=["mf2"], w=[bk(sbank), tkey])
                        late.append((lambda cs=cs, tb=tb, pr=pr, wd=wd: A.activation(out=et[:, cs], in_=tb[:, 0:wd], func=AF.Exp,
                                                                                      bias=fbt[:, h, j, 2 * pr:2 * pr + 1]),
                                     [tkey, ("fbt", h)]))
                    elif merged_pair[0] == pr:
                        continue
                    elif runs and runs[-1][2] == pr and runs[-1][1] == bi * 128:
                        runs[-1] = (runs[-1][0], (bi + 1) * 128, pr)
                    else:
                        runs.append((bi * 128, (bi + 1) * 128, pr))
                for (lo_, hi_, pr) in runs:
                    T.op("act", lambda lo_=lo_, hi_=hi_, pr=pr: A.activation(out=et[:, lo_:hi_], in_=ps[sbank][:, lo_:hi_], func=AF.Exp,
                                                                            bias=fbt[:, h, j, 2 * pr:2 * pr + 1]),
                         r=[("fbt", h)], w=[bk(sbank), ("ET", etb)])
            for fn, rk_ in late:
                T.op("act", fn, r=rk_, w=[("ET", etb)])
            return (etb, i0, nb)

        def accb(I, c):
            return (3, 4)

        def emit_AV(n, info):
            I, c, j = steps[n]
            etb, i0, nb = info
            et = ET[etb]
            ncol = nb * 128
            c0 = (i0 - 4 * I) * 128
            first = (j == 0)
            last = (j == 4 * I + 3)

            bA, bB = accb(I, c)

            def mm():
                PE.matmul(ps[bA][:, c0:c0 + ncol], lhsT=vv[:, j, :], rhs=et[:, 0:ncol], start=first, stop=last)
                return PE.matmul(ps[bB][:, c0:c0 + ncol], lhsT=oneb[:], rhs=et[:, 0:ncol], start=first, stop=last)
            T.op("pe", mm, r=[("ET", etb), ("vb", slot, j // 4), "oneb"], w=[bk(bA), bk(bB)])
            if last:
                finalize(I, c)

        o1f = o1[:].rearrange("p a b -> p (a b)")
        obfs = [ob[:].rearrange("p a b -> p (a b)"), obB[:].rearrange("p a b -> p (a b)")]
        obnfs = [obn[:].rearrange("p a b -> p (a b)"), obnB[:].rearrange("p a b -> p (a b)")]

        def finalize(I, c):
            bA, bB = accb(I, c)
            T.op("act", lambda: A.activation(out=rB[:], in_=ps[bB][:], func=AF.Ln), w=[bk(bB), "rB"])
            T.op("act", lambda: A.activation(out=rB[:], in_=rB[:], func=AF.Exp, scale=-1.0), w=["rB"])
            if isdiff and c == 0:
                T.op("dve", lambda: V.tensor_tensor(out=o1f, in0=ps[bA][:], in1=rB[:], op=ALU.mult), r=["rB"], w=[bk(bA), "o1"])
                return
            par = fin_cnt[0] % 2
            fin_cnt[0] += 1
            obf, obnf, rr = obfs[par], obnfs[par], rB2[par]
            T.op("dve", lambda: V.tensor_tensor(out=obf, in0=ps[bA][:], in1=rB[:], op=ALU.mult), r=["rB"], w=[bk(bA), ("ob", par)])
            if isdiff:
                T.op("dve", lambda: V.scalar_tensor_tensor(out=obf, in0=obf, scalar=lams[:, 5:6], in1=o1f, op0=ALU.mult, op1=ALU.add),
                     r=["neglam", "o1"], w=[("ob", par)])
            T.op("dve", lambda: V.tensor_tensor(out=obnf, in0=obf, in1=obf, op=ALU.mult), r=[("ob", par)], w=[("obn", par)])

            def part2():
                sbk = next_gbank()
                T.op("pe", lambda: PE.matmul(ps[sbk][:], lhsT=oneb[:], rhs=obnf, start=True, stop=True), r=[("obn", par), "oneb"], w=[bk(sbk)])
                T.op("act", lambda: A.activation(out=rr[:], in_=ps[sbk][:], func=AF.Ln, scale=1.0 / 128, bias=EPS), w=[bk(sbk), ("rB2", par)])
                T.op("act", lambda: A.activation(out=rr[:], in_=rr[:], func=AF.Exp, scale=-0.5), w=[("rB2", par)])
                gcol = pp[:, 4:5] if isdiff else pp[:, 5:6]
                T.op("dve", lambda: V.scalar_tensor_tensor(out=dstT[:, H, I * 512:(I + 1) * 512], in0=obf, scalar=gcol, in1=rr[:],
                                                           op0=ALU.mult, op1=ALU.mult),
                     r=[("ob", par), ("rB2", par), "pp4", "pp5"], w=[("mixT", H, I)])
            deferred.append([6, part2])

        return len(steps), emit_S, emit_AV

    deferred = []

    order = [4, 5, 6, 7, 0, 1, 2, 3]
    pc0 = proj_chunks(order[0], pbanks=(5, 3, 4))
    qa = [pc0[1 + 2 * k] for k in range(8)]
    qb = [pc0[2 + 2 * k] for k in range(8)]
    qa[0]()
    for k in range(1, 8):
        qa[k]()
        qb[k - 1]()
    qb[7]()
    for ch in pc0[17:]:
        ch()
    heads = {}
    gsteps = []
    pend_of = {}
    for oi, H in enumerate(order):
        nst_h, eS, eAV = attention(H, None, mixT)
        heads[H] = (eS, eAV)
        gsteps += [(H, n) for n in range(nst_h)]
        pend = proj_chunks(order[oi + 1]) if oi < 7 else []
        if oi < 4:
            extra = bt_chunks[4 * oi:4 * oi + 4] + [mem_chunks[oi]] + ([bt_finish] if oi == 3 else [])
            merged = [pend.pop(0)]
            while pend or extra:
                if pend:
                    merged.append(pend.pop(0))
                if pend:
                    merged.append(pend.pop(0))
                if extra:
                    merged.append(extra.pop(0))
            pend = merged
        pend_of[H] = (pend, max(1, nst_h // (len(pend) + 1)) if pend else nst_h)
    infos = {}
    DEPTH = 3
    curH = None
    for g in range(len(gsteps) + DEPTH):
        if g < len(gsteps):
            H, n = gsteps[g]
            if H != curH:
                if curH is not None:
                    while pend_of[curH][0]:
                        pend_of[curH][0].pop(0)()
                curH = H
            infos[g] = heads[H][0](n)
        if g >= DEPTH:
            H2, n2 = gsteps[g - DEPTH]
            heads[H2][1](n2, infos.pop(g - DEPTH))
        for d in list(deferred):
            d[0] -= 1
            if d[0] <= 0:
                deferred.remove(d)
                d[1]()
        if g < len(gsteps):
            pend, every = pend_of[gsteps[g][0]]
            if pend and gsteps[g][1] % every == every - 1:
                pend.pop(0)()
    for d in deferred:
        d[1]()
    T.barrier()
    hs.close()
    xres = sb("xres", [128, NT, D])
    for t in range(NT):
        T.dma("sp", xres[:, t, :], x_d[t * 128:(t + 1) * 128, :], w=[("xres", t)])

    def load_w_full(w_d, slots, key):
        for hf in range(2):
            T.dma("pool", wring[slots[hf]][:].rearrange("p (c n) -> p c n", n=512), wv(w_d)[:, :, hf * 512:(hf + 1) * 512],
                  w=[("wr", slots[hf])])

    def proj_residual(srcT, skeyf, slots, wkey, tiles=range(NT)):
        bi = 0
        for t in tiles:
            for hf in range(2):
                bnk = bi % 3
                bi += 1
                wsl = wring[slots[hf]][:].rearrange("p (c n) -> p c n", n=512)

                def mm(t=t, wsl=wsl, bnk=bnk):
                    for c in range(8):
                        i = PE.matmul(ps[bnk][:], lhsT=srcT[:, c, t * 128:(t + 1) * 128], rhs=wsl[:, c, :], start=(c == 0), stop=(c == 7))
                    return i
                T.op("pe", mm, r=[("wr", slots[hf])] + skeyf(t), w=[bk(bnk)])
                T.op("dve", lambda t=t, hf=hf, bnk=bnk: V.tensor_tensor(out=xres[:, t, hf * 512:(hf + 1) * 512], in0=ps[bnk][:],
                                                                       in1=xres[:, t, hf * 512:(hf + 1) * 512], op=ALU.add),
                     w=[bk(bnk), ("xres", t)])

    load_w_full(w_out_d, (0, 1), "wout")
    wout_keys = lambda t: [("mixT", H, t // 4) for H in range(8)]
    proj_residual(mixT, wout_keys, (0, 1), "wout", tiles=range(0, 8))

    hT2 = sb("hT2", [128, 8, S], BF16)
    PHB = {}

    def pre_b(t):
        def f():
            if t < 4:
                proj_residual(mixT, wout_keys, (0, 1), "wout", tiles=[8 + 2 * t, 9 + 2 * t])
            q0 = PHB.get("q0")
            if t == 4:
                q0[0]()
            if q0 is not None:
                tgq, r = divmod(t - 5, 4)
                if t >= 5 and r == 0 and tgq < 3:
                    q0[1 + 2 * tgq]()
                if t >= 7 and (t - 7) % 4 == 0 and (t - 7) // 4 < 3:
                    q0[2 + 2 * ((t - 7) // 4)]()
        return f
    PHB["pre_b"] = pre_b

    def hT2_keys(tg):
        return [("hT2", tg, c) for c in range(8)]

    crossT = mixT
    gb_list[:] = [0, 1, 2]
    rBc = xn_bufs[0][:].bitcast(F32)

    def qproj_chunks(h, ob=(6, 7)):
        slot = h % 2
        wsl = wring[slot][:, 0:2048].rearrange("p (c n) -> p c n", n=256)
        qc = qk[slot]
        chunks = []

        def load():
            T.dma("pool", wsl, wv(w_cq_d)[:, :, h * 256:(h + 1) * 256], w=[("wr", slot)])
        chunks.append(load)
        for tg in range(4):
            holder = []

            def cha(tg=tg, holder=holder):
                def mmq():
                    for b in range(2):
                        for c in range(8):
                            i = PE.matmul(ps[ob[b]][:], lhsT=wsl[:, c, b * 128:(b + 1) * 128], rhs=hT2[:, c, tg * 512:(tg + 1) * 512],
                                          start=(c == 0), stop=(c == 7))
                    return i
                T.op("pe", mmq, r=[("wr", slot)] + hT2_keys(tg), w=[bk(ob[0]), bk(ob[1])])
                emit_qknorm([ob[0], ob[1]], 256, [pp[:, 6:7], pp[:, 7:8]], [qc[:, 0, tg * 512:(tg + 1) * 512], qc[:, 1, tg * 512:(tg + 1) * 512]],
                            ["pp6"], [[("qk", slot, 0, tg)], [("qk", slot, 1, tg)]], oneb[:], n=512, defer=holder)
            chunks.append(cha)
            chunks.append(lambda holder=holder: holder.pop(0)())
        return chunks

    def cross_attention(h, pending):
        slot = h % 2
        qc = qk[slot]

        def emit_S(tg):
            ets = []
            for mb in range(2):
                sbank = next_gbank()
                etb = st_cnt[0] % 4
                st_cnt[0] += 1
                ets.append(etb)

                def mms(mb=mb, sbank=sbank):
                    for b in range(2):
                        i = PE.matmul(ps[sbank][:], lhsT=kTc[:, h, b, mb * 128:(mb + 1) * 128], rhs=qc[:, b, tg * 512:(tg + 1) * 512],
                                      start=(b == 0), stop=(b == 1))
                    return i
                T.op("pe", mms, r=[("kTc", h), ("qk", slot, 0, tg), ("qk", slot, 1, tg)], w=[bk(sbank)])
                T.op("act", lambda sbank=sbank, etb=etb: A.activation(out=ET[etb][:], in_=ps[sbank][:], func=AF.Exp), w=[bk(sbank), ("ET", etb)])
            return ets

        def emit_AV(tg, ets):
            def mma():
                for b in range(2):
                    for mb in range(2):
                        PE.matmul(ps[3 + b][:], lhsT=vc[:, mb, h, b * 128:(b + 1) * 128], rhs=ET[ets[mb]][:], start=(mb == 0), stop=(mb == 1))
                for mb in range(2):
                    i = PE.matmul(ps[5][:], lhsT=oneb[:], rhs=ET[ets[mb]][:], start=(mb == 0), stop=(mb == 1))
                return i
            T.op("pe", mma, r=[("ET", ets[0]), ("ET", ets[1]), ("vc", 0, h // 2), ("vc", 1, h // 2), "oneb"], w=[bk(3), bk(4), bk(5)])
            T.op("act", lambda: A.activation(out=rBc, in_=ps[5][:], func=AF.Ln), w=[bk(5), ("xn", 0)])
            T.op("act", lambda: A.activation(out=rBc, in_=rBc, func=AF.Exp, scale=-1.0), w=[("xn", 0)])
            for b in range(2):
                T.op("dve", lambda b=b: V.tensor_tensor(out=crossT[:, 2 * h + b, tg * 512:(tg + 1) * 512], in0=ps[3 + b][:], in1=rBc, op=ALU.mult),
                     r=[("xn", 0)], w=[bk(3 + b), ("mixT", 2 * h + b, tg)])

        return emit_S, emit_AV

    q0 = qproj_chunks(0, ob=(3, 4))
    PHB["q0"] = q0
    norm_all([(t, xres[:, t, :], [("xres", t)], PHB["pre_b"](t)) for t in range(NT)], norm_cross_g_d, hT2, "hT2")
    for ch in q0[7:]:
        ch()
    wco_keys = lambda t: [("mixT", cb, t // 4) for cb in range(8)]
    xh = {h: cross_attention(h, None) for h in range(4)}
    xpend = {h: (qproj_chunks(h + 1) if h < 3 else []) for h in range(4)}
    xsteps = [(h, tg) for h in range(4) for tg in range(4)]
    xinfos = {}
    for n in range(len(xsteps) + 1):
        if n < len(xsteps):
            h, tg = xsteps[n]
            if tg == 0 and h > 0:
                while xpend[h - 1]:
                    xpend[h - 1].pop(0)()
            if tg == 0 and h == 3:
                load_w_full(w_co_d, (0, 1), "wco")
            xinfos[n] = xh[h][0](tg)
            for _ in range(3):
                if xpend[h]:
                    xpend[h].pop(0)()
        if n >= 1:
            h2, tg2 = xsteps[n - 1]
            xh[h2][1](tg2, xinfos.pop(n - 1))
            if h2 == 3 and tg2 >= 1:
                proj_residual(crossT, wco_keys, (0, 1), "wco", tiles=range(4 * (tg2 - 1), 4 * (tg2 - 1) + 4))
    proj_residual(crossT, wco_keys, (0, 1), "wco", tiles=range(12, 16))

    T.barrier()
    norm_all([(t, xres[:, t, :], [("xres", t)], None) for t in range(NT)], norm_ffn_g_d, hT2, "hT2")
    wrt = wring[2]
    T.dma("pool", wrt[:, 0:256].rearrange("p (c n) -> p c n", n=32)[:, :, 0:4], wv(w_gr_d), w=[("wr", 2)], allow_slow_non_contiguous=True)
    T.dma("pool", wrt[:, 0:256].rearrange("p (c n) -> p c n", n=32)[:, :, 4:20], wv(w_er_d), w=[("wr", 2)], allow_slow_non_contiguous=True)

    def mmr():
        for t in range(NT):
            for c in range(8):
                i = PE.matmul(ps[0][:, 32 * t:32 * t + 20], lhsT=hT2[:, c, t * 128:(t + 1) * 128], rhs=wrt[:, 32 * c:32 * c + 20],
                              start=(c == 0), stop=(c == 7))
        return i
    T.op("pe", mmr, r=[("wr", 2)] + [k for tg in range(4) for k in hT2_keys(tg)], w=[bk(0)])
    T.op("dve", lambda: V.tensor_tensor(out=rl[:], in0=ps[0][:].rearrange("p (t n) -> p t n", n=32)[:, :, 0:20],
                                        in1=b20[:].unsqueeze(1).broadcast_to([128, NT, 20]), op=ALU.add),
         r=["b20a", "b20b"], w=[bk(0), "rl"])
    RS = rsb[0][:].rearrange("p (t n) -> p t n", n=32)
    z = rl

    def bc(ap2):
        return ap2.unsqueeze(2).broadcast_to([128, NT, 4])

    def dv(fn):
        T.op("dve", fn, r=["rl"], w=["rtr"])

    def ac(fn):
        T.op("act", fn, r=["rl"], w=["rtr"])
    gmax, ngm, sumg, pg = RS[:, :, 0], RS[:, :, 1], RS[:, :, 2], RS[:, :, 3]
    mg, eg, sel, m1, sel2, m2, gin, tmp4 = (RS[:, :, 4 + 4 * i:8 + 4 * i] for i in range(7)) if False else tuple(RS[:, :, 4 + 4 * i:8 + 4 * i] for i in range(7)) + (None,)
    v1, v2, dd, e2 = rt[:, 0:16], rt[:, 16:32], rt[:, 32:48], rt[:, 48:64]
    dv(lambda: V.tensor_reduce(out=gmax, in_=z[:, :, 0:4], axis=AX.X, op=ALU.max))
    dv(lambda: V.tensor_tensor(out=mg, in0=z[:, :, 0:4], in1=bc(gmax), op=ALU.is_equal))
    dv(lambda: V.tensor_tensor(out=eg, in0=z[:, :, 0:4], in1=bc(gmax), op=ALU.subtract))
    ac(lambda: A.activation(out=eg, in_=eg, func=AF.Exp))
    dv(lambda: V.tensor_reduce(out=sumg, in_=eg, axis=AX.X, op=ALU.add))
    dv(lambda: V.reciprocal(out=pg, in_=sumg))
    dv(lambda: V.tensor_tensor(out=sel, in0=z[:, :, 4:8], in1=bc(mg[:, :, 0]), op=ALU.mult))
    for g in range(1, 4):
        dv(lambda g=g: V.tensor_tensor(out=sel2, in0=z[:, :, 4 + 4 * g:8 + 4 * g], in1=bc(mg[:, :, g]), op=ALU.mult))
        dv(lambda: V.tensor_tensor(out=sel, in0=sel, in1=sel2, op=ALU.add))
    dv(lambda: V.tensor_reduce(out=v1, in_=sel, axis=AX.X, op=ALU.max))
    dv(lambda: V.tensor_tensor(out=m1, in0=sel, in1=bc(v1), op=ALU.is_equal))
    dv(lambda: V.scalar_tensor_tensor(out=sel2, in0=m1, scalar=-1e30, in1=sel, op0=ALU.mult, op1=ALU.add))
    dv(lambda: V.tensor_reduce(out=v2, in_=sel2, axis=AX.X, op=ALU.max))
    dv(lambda: V.tensor_tensor(out=m2, in0=sel2, in1=bc(v2), op=ALU.is_equal))
    dv(lambda: V.tensor_tensor(out=dd, in0=v2, in1=v1, op=ALU.subtract))
    ac(lambda: A.activation(out=e2, in_=dd, func=AF.Exp))
    dv(lambda: V.tensor_scalar(out=dd, in0=e2, scalar1=1.0, scalar2=None, op0=ALU.add))
    dv(lambda: V.reciprocal(out=dd, in_=dd))
    dv(lambda: V.tensor_tensor(out=v1, in0=dd, in1=pg, op=ALU.mult))
    dv(lambda: V.tensor_tensor(out=v2, in0=v1, in1=e2, op=ALU.mult))
    dv(lambda: V.tensor_tensor(out=gin, in0=m1, in1=bc(v1), op=ALU.mult))
    dv(lambda: V.tensor_tensor(out=sel2, in0=m2, in1=bc(v2), op=ALU.mult))
    dv(lambda: V.tensor_tensor(out=gin, in0=gin, in1=sel2, op=ALU.add))
    for g in range(4):
        T.op("dve", lambda g=g: V.tensor_tensor(out=gates[:, :, 4 * g:4 * g + 4], in0=gin, in1=bc(mg[:, :, g]), op=ALU.mult),
             r=["rtr"], w=[("gates", t) for t in range(NT)])

    mflat = mixT[:].rearrange("p a b -> p (a b)")
    wslots = [wring[0][:], wring[1][:], wring[2][:], mflat[:, 0:4096], mflat[:, 4096:8192], mflat[:, 8192:12288]]
    aTb = [mflat[:, 12288:14336].rearrange("p (f n) -> p f n", n=512), mflat[:, 14336:16384].rearrange("p (f n) -> p f n", n=512)]
    sil = [sqb[0], sqb[1]]

    def wslot(e, k):
        return wslots[(e % 2) * 3 + k]

    def wxk(i):
        return ("wr", i) if i < 3 else ("wx", i)

    def load_expert(e):
        s3 = (e % 2) * 3
        T.dma("pool", wslot(e, 0).rearrange("p (c n) -> p c n", n=512), wv(w_eg_d[e]), w=[wxk(s3)])
        T.dma("pool", wslot(e, 1).rearrange("p (c n) -> p c n", n=512), wv(w_eu_d[e]), w=[wxk(s3 + 1)])
        T.dma("pool", wslot(e, 2).rearrange("p (c n) -> p c n", n=1024), wv(w_ed_d[e]), w=[wxk(s3 + 2)])

    gu_cnt = [0]

    def emit_GU(e, tg):
        s3 = (e % 2) * 3
        wg = wslot(e, 0).rearrange("p (c n) -> p c n", n=512)
        wu = wslot(e, 1).rearrange("p (c n) -> p c n", n=512)
        ab = (e * 4 + tg) % 2
        for fb in range(4):
            k = gu_cnt[0] % 2
            gu_cnt[0] += 1
            gb, ub = k, 2 + k

            def mmg(fb=fb, gb=gb):
                for c in range(8):
                    i = PE.matmul(ps[gb][:], lhsT=wg[:, c, fb * 128:(fb + 1) * 128], rhs=hT2[:, c, tg * 512:(tg + 1) * 512], start=(c == 0), stop=(c == 7))
                return i

            def mmu(fb=fb, ub=ub):
                for c in range(8):
                    i = PE.matmul(ps[ub][:], lhsT=wu[:, c, fb * 128:(fb + 1) * 128], rhs=hT2[:, c, tg * 512:(tg + 1) * 512], start=(c == 0), stop=(c == 7))
                return i
            T.op("pe", mmg, r=[wxk(s3)] + hT2_keys(tg), w=[bk(gb)])
            T.op("pe", mmu, r=[wxk(s3 + 1)] + hT2_keys(tg), w=[bk(ub)])
            T.op("act", lambda gb=gb, k=k: A.activation(out=sil[k][:], in_=ps[gb][:], func=AF.Silu), w=[bk(gb), ("sil", k)])
            T.op("dve", lambda fb=fb, ub=ub, k=k: V.tensor_tensor(out=aTb[ab][:, fb, :], in0=sil[k][:], in1=ps[ub][:], op=ALU.mult),
                 r=[("sil", k)], w=[bk(ub), ("aT", ab, fb)])

    d_cnt = [0]

    def emit_D(e, tg):
        s3 = (e % 2) * 3
        wd = wslot(e, 2).rearrange("p (c n) -> p c n", n=1024)
        ab = (e * 4 + tg) % 2
        for tt in range(4):
            t = tg * 4 + tt
            for hf in range(2):
                bnk = 4 + d_cnt[0] % 3
                d_cnt[0] += 1

                def mmd(tt=tt, hf=hf, bnk=bnk):
                    for fb in range(4):
                        i = PE.matmul(ps[bnk][:], lhsT=aTb[ab][:, fb, tt * 128:(tt + 1) * 128], rhs=wd[:, fb, hf * 512:(hf + 1) * 512],
                                      start=(fb == 0), stop=(fb == 3))
                    return i
                T.op("pe", mmd, r=[wxk(s3 + 2)] + [("aT", ab, fb) for fb in range(4)], w=[bk(bnk)])
                T.op("dve", lambda t=t, hf=hf, bnk=bnk: V.scalar_tensor_tensor(out=xres[:, t, hf * 512:(hf + 1) * 512], in0=ps[bnk][:],
                                                                              scalar=gates[:, t, e:e + 1], in1=xres[:, t, hf * 512:(hf + 1) * 512],
                                                                              op0=ALU.mult, op1=ALU.add),
                     r=[("gates", t)], w=[bk(bnk), ("xres", t)])
            if e == 15:
                T.dma("sp", out_d[t * 128:(t + 1) * 128, :], xres[:, t, :], r=[("xres", t)], w=[("out", t)])

    load_expert(0)
    stepsC = [(e, tg) for e in range(16) for tg in range(4)]
    for n in range(len(stepsC) + 1):
        if n < len(stepsC):
            e, tg = stepsC[n]
            if tg == 0 and e + 1 < 16:
                pass
            emit_GU(e, tg)
            if tg == 1 and e + 1 < 16:
                load_expert(e + 1)
        if n >= 1:
            emit_D(*stepsC[n - 1])

    T.finish("sp")
    es.close()
    T.close()
    return nc, T


_CACHE = {}


def kernel(**inputs):
    if "nc" not in _CACHE:
        _CACHE["nc"] = build_program()[0]
    nc = _CACHE["nc"]
    cst = _make_consts()
    f32 = lambda a: np.ascontiguousarray(np.asarray(a, dtype=np.float32))
    shared = {"cst": cst}
    for k, v in inputs.items():
        if k in ("x", "mem"):
            continue
        a = f32(v)
        if k == "rel_bias":
            shared[k] = a
        else:
            shared[k] = np.ascontiguousarray(a[0])
    x = f32(inputs["x"])
    mem = f32(inputs["mem"])
    in_maps = []
    for b in range(8):
        m = dict(shared)
        m["x"] = np.ascontiguousarray(x[b])
        m["mem"] = np.ascontiguousarray(mem[b])
        in_maps.append(m)
    res = run_bass_kernel_spmd(nc, in_maps, core_ids=list(range(8)))
    out = np.stack([np.asarray(r["out"], dtype=np.float32) for r in res.results], axis=0)
    return out
```
